# Optimizing a Trainium2 kernel written in Bass

```python
import jax, jax.numpy as jnp
from jax import lax
import numpy as np

D_MODEL = 1024
BATCH = 2
SEQ = 8192
DEPTH = 1

HEAD_DIM = 64
N_HEADS_DSA = 8
N_HEADS_FOX = 8
N_HEADS_IDX = 8
IDX_DIM = 64
TOPK_MAX = 256
Q_BLOCK = 128
D_FF = 2816
N_MOD = 9
EPS = 1e-6
FORGET_BIAS_CENTER = 3.0

WIDTH_DSA = N_HEADS_DSA * HEAD_DIM
WIDTH_FOX = N_HEADS_FOX * HEAD_DIM
IN_SIZES = (WIDTH_DSA, WIDTH_DSA, WIDTH_DSA, WIDTH_FOX, WIDTH_FOX, WIDTH_FOX,
            N_HEADS_IDX * IDX_DIM, IDX_DIM, N_HEADS_IDX, N_HEADS_FOX, D_MODEL, D_MODEL)
IN_WIDTH = 3 * WIDTH_DSA + 3 * WIDTH_FOX + N_HEADS_IDX * IDX_DIM + IDX_DIM + N_HEADS_IDX + N_HEADS_FOX + 2 * D_MODEL

kernel_name = "hybrid_dsa_fox_macaron_adaln_layer"


def rms_norm(x, g):
    xf = x.astype(jnp.float32)
    y = xf * lax.rsqrt(jnp.mean(xf * xf, axis=-1, keepdims=True) + EPS)
    return (y * g.astype(jnp.float32)).astype(x.dtype)


def swiglu(h, wg, wu, wd):
    return (jax.nn.silu(h @ wg) * (h @ wu)) @ wd


def split_columns(z):
    parts, start = [], 0
    for width in IN_SIZES:
        parts.append(z[..., start:start + width])
        start += width
    return parts


def alibi_slopes(n):
    return jnp.exp2(-8.0 * jnp.arange(1, n + 1, dtype=jnp.float32) / n)


def to_blocks(a):
    b, s = a.shape[0], a.shape[1]
    return jnp.moveaxis(a.reshape(b, s // Q_BLOCK, Q_BLOCK, *a.shape[2:]), 1, 0)


def from_blocks(a):
    a = jnp.moveaxis(a, 0, 1)
    return a.reshape(a.shape[0], a.shape[1] * a.shape[2], *a.shape[3:])


def dsa_attention(q, k, v, q_idx, k_idx, w_idx):
    b, s, h, dh = q.shape
    n_keys = s
    topk = min(TOPK_MAX, n_keys // 4)
    slopes = alibi_slopes(h)
    s_pos = jnp.arange(s)
    k_idx32 = k_idx.astype(jnp.float32)

    def block_fn(args):
        blk, qb, qib, wib = args
        t_pos = blk * Q_BLOCK + jnp.arange(Q_BLOCK)
        rel = jax.nn.relu(jnp.einsum('btid,bsd->btis', qib.astype(jnp.float32), k_idx32))
        score = jnp.einsum('btis,bti->bts', rel, wib.astype(jnp.float32))
        causal = s_pos[None, :] <= t_pos[:, None]
        score = jnp.where(causal[None], score, -jnp.inf)
        _, sel = lax.top_k(score, topk)
        flat = sel.reshape(b, Q_BLOCK * topk)
        k_sel = jax.vmap(lambda kk, ii: kk[ii])(k, flat).reshape(b, Q_BLOCK, topk, h, dh)
        v_sel = jax.vmap(lambda vv, ii: vv[ii])(v, flat).reshape(b, Q_BLOCK, topk, h, dh)
        logits = jnp.einsum('bthd,btkhd->bthk', qb, k_sel).astype(jnp.float32)
        dist = (t_pos[None, :, None] - sel).astype(jnp.float32)
        logits = logits - slopes[None, None, :, None] * dist[:, :, None, :]
        valid = sel <= t_pos[None, :, None]
        logits = jnp.where(valid[:, :, None, :], logits, -jnp.inf)
        p = jax.nn.softmax(logits, axis=-1).astype(v.dtype)
        return jnp.einsum('bthk,btkhd->bthd', p, v_sel)

    nb = s // Q_BLOCK
    out = lax.map(block_fn, (jnp.arange(nb), to_blocks(q), to_blocks(q_idx), to_blocks(w_idx)))
    return from_blocks(out)


def fox_attention(q, k, v, log_f):
    b, s, h, dh = q.shape
    F = jnp.cumsum(log_f, axis=1)
    F_keys = jnp.swapaxes(F, 1, 2)
    s_pos = jnp.arange(s)

    def block_fn(args):
        blk, qb, Fb = args
        t_pos = blk * Q_BLOCK + jnp.arange(Q_BLOCK)
        logits = jnp.einsum('bthd,bshd->bhts', qb, k).astype(jnp.float32)
        logits = logits + jnp.swapaxes(Fb, 1, 2)[..., None] - F_keys[:, :, None, :]
        causal = s_pos[None, :] <= t_pos[:, None]
        logits = jnp.where(causal[None, None], logits, -jnp.inf)
        p = jax.nn.softmax(logits, axis=-1).astype(v.dtype)
        return jnp.einsum('bhts,bshd->bthd', p, v)

    nb = s // Q_BLOCK
    out = lax.map(block_fn, (jnp.arange(nb), to_blocks(q), to_blocks(F)))
    return from_blocks(out)


def token_mix(h, w_in, b_forget, qn_dsa, kn_dsa, qn_fox, kn_fox, w_br_dsa, w_br_fox, w_out):
    b, s, _ = h.shape
    z = h @ w_in
    (qa, ka, va, qf, kf, vf, qi, ki, wi, fpre, ga, gb) = split_columns(z)
    scale = HEAD_DIM ** -0.5
    qa = rms_norm(qa.reshape(b, s, N_HEADS_DSA, HEAD_DIM), qn_dsa) * scale
    ka = rms_norm(ka.reshape(b, s, N_HEADS_DSA, HEAD_DIM), kn_dsa)
    va = va.reshape(b, s, N_HEADS_DSA, HEAD_DIM)
    qf = rms_norm(qf.reshape(b, s, N_HEADS_FOX, HEAD_DIM), qn_fox) * scale
    kf = rms_norm(kf.reshape(b, s, N_HEADS_FOX, HEAD_DIM), kn_fox)
    vf = vf.reshape(b, s, N_HEADS_FOX, HEAD_DIM)
    qi = qi.reshape(b, s, N_HEADS_IDX, IDX_DIM) * (IDX_DIM ** -0.5)
    wi = wi * (N_HEADS_IDX ** -0.5)
    log_f = jax.nn.log_sigmoid(fpre.astype(jnp.float32) + b_forget.astype(jnp.float32))

    y_dsa = dsa_attention(qa, ka, va, qi, ki, wi).reshape(b, s, WIDTH_DSA) @ w_br_dsa
    y_fox = fox_attention(qf, kf, vf, log_f).reshape(b, s, WIDTH_FOX) @ w_br_fox
    merged = jax.nn.sigmoid(ga) * y_dsa + jax.nn.sigmoid(gb) * y_fox
    return merged @ w_out


def setup_inputs(seed: int = 0) -> dict:
    key = jax.random.key(seed)
    ks = jax.random.split(key, 24)
    f32 = jnp.float32

    def w(k, shape, fan_in, scale=1.0):
        return scale * (fan_in ** -0.5) * jax.random.normal(k, shape, f32)

    def gain(k, shape):
        return 1.0 + 0.02 * jax.random.normal(k, shape, f32)

    L = DEPTH
    return {
        "x": jax.random.normal(ks[0], (BATCH, SEQ, D_MODEL), f32),
        "c": jax.random.normal(ks[1], (BATCH, D_MODEL), f32),
        "ada_w": w(ks[2], (L, D_MODEL, N_MOD * D_MODEL), D_MODEL, 0.5),
        "ada_b": 0.02 * jax.random.normal(ks[3], (L, N_MOD * D_MODEL), f32),
        "norm1_g": gain(ks[4], (L, D_MODEL)),
        "ffn1_wg": w(ks[5], (L, D_MODEL, D_FF), D_MODEL),
        "ffn1_wu": w(ks[6], (L, D_MODEL, D_FF), D_MODEL),
        "ffn1_wd": w(ks[7], (L, D_FF, D_MODEL), D_FF),
        "norm2_g": gain(ks[8], (L, D_MODEL)),
        "w_in": w(ks[9], (L, D_MODEL, IN_WIDTH), D_MODEL),
        "b_forget": FORGET_BIAS_CENTER + 0.5 * jax.random.normal(ks[10], (L, N_HEADS_FOX), f32),
        "qn_dsa": gain(ks[11], (L, HEAD_DIM)),
        "kn_dsa": gain(ks[12], (L, HEAD_DIM)),
        "qn_fox": gain(ks[13], (L, HEAD_DIM)),
        "kn_fox": gain(ks[14], (L, HEAD_DIM)),
        "w_br_dsa": w(ks[15], (L, WIDTH_DSA, D_MODEL), WIDTH_DSA),
        "w_br_fox": w(ks[16], (L, WIDTH_FOX, D_MODEL), WIDTH_FOX),
        "w_out": w(ks[17], (L, D_MODEL, D_MODEL), D_MODEL),
        "norm3_g": gain(ks[18], (L, D_MODEL)),
        "ffn2_wg": w(ks[19], (L, D_MODEL, D_FF), D_MODEL),
        "ffn2_wu": w(ks[20], (L, D_MODEL, D_FF), D_MODEL),
        "ffn2_wd": w(ks[21], (L, D_FF, D_MODEL), D_FF),
    }


def reference(x, c, ada_w, ada_b, norm1_g, ffn1_wg, ffn1_wu, ffn1_wd, norm2_g, w_in, b_forget,
              qn_dsa, kn_dsa, qn_fox, kn_fox, w_br_dsa, w_br_fox, w_out, norm3_g,
              ffn2_wg, ffn2_wu, ffn2_wd):
    b = x.shape[0]
    for l in range(DEPTH):
        mod = (jax.nn.silu(c) @ ada_w[l] + ada_b[l]).reshape(b, N_MOD, 1, D_MODEL)
        sh1, sc1, g1 = mod[:, 0], mod[:, 1], mod[:, 2]
        sh2, sc2, g2 = mod[:, 3], mod[:, 4], mod[:, 5]
        sh3, sc3, g3 = mod[:, 6], mod[:, 7], mod[:, 8]
        h = rms_norm(x, norm1_g[l]) * (1.0 + sc1) + sh1
        x = x + 0.5 * g1 * swiglu(h, ffn1_wg[l], ffn1_wu[l], ffn1_wd[l])
        h = rms_norm(x, norm2_g[l]) * (1.0 + sc2) + sh2
        x = x + g2 * token_mix(h, w_in[l], b_forget[l], qn_dsa[l], kn_dsa[l], qn_fox[l], kn_fox[l],
                               w_br_dsa[l], w_br_fox[l], w_out[l])
        h = rms_norm(x, norm3_g[l]) * (1.0 + sc3) + sh3
        x = x + 0.5 * g3 * swiglu(h, ffn2_wg[l], ffn2_wu[l], ffn2_wd[l])
    return x
```

```python
from contextlib import ExitStack
import numpy as np
import ml_dtypes
import concourse.bass as bass
import concourse.mybir as mybir
from concourse.bass_utils import run_bass_kernel_spmd

F32 = mybir.dt.float32
BF16 = mybir.dt.bfloat16
AF = mybir.ActivationFunctionType
ALU = mybir.AluOpType
AX = mybir.AxisListType

D = 1024
S = 8192
NT = 2048
DFF = 2816
NJ = 22
INW = 5712
EPS = 1e-6
NIT = 16
TOPK = 256
NEG = -30000.0
NEGA = -240.0

STAGES = 9
import os as _os
DBG = int(_os.environ.get("KDBG", "0"))


class T:
    __slots__ = ("w", "r")

    def __init__(self):
        self.w = None
        self.r = {}


def Ts(n):
    return [T() for _ in range(n)]


class Prog:
    ENG = ["pe", "act", "dve", "pool", "sp"]

    def __init__(self, nc, sems):
        self.nc = nc
        self.sem = sems
        self.cnt = {k: 0 for k in sems}
        self.seen = {n: {} for n in self.ENG}
        self.q = {n: [] for n in self.ENG}
        self.dn = {}

    def _wait(self, eng, key, val):
        if val <= 0 or self.seen[eng].get(key, 0) >= val:
            return
        self.seen[eng][key] = val
        sem = self.sem[key]
        self.q[eng].append(lambda e, sem=sem, val=val: e.wait_ge(sem, val))

    def _deps(self, eng, reads, writes, selfkey):
        for b in reads:
            if b.w is not None and not (b.w[0] == "pe" and selfkey == "pe"):
                self._wait(eng, *b.w)
        for b in writes:
            if b.w is not None and not (b.w[0] == "pe" and selfkey == "pe"):
                self._wait(eng, *b.w)
            for k, v in b.r.items():
                if not (k == "pe" and selfkey == "pe"):
                    self._wait(eng, k, v)

    def op(self, eng, fn, reads=(), writes=()):
        self._deps(eng, reads, writes, eng)
        self.cnt[eng] += 1
        c = self.cnt[eng]
        sem = self.sem[eng]
        self.q[eng].append(lambda e, fn=fn, sem=sem: fn(e).then_inc(sem, 1))
        for b in reads:
            b.r[eng] = c
        for b in writes:
            b.w = (eng, c)
            b.r = {}

    NDS = 16

    def dma(self, eng, fn, reads=(), writes=()):
        n = self.dn.get(eng, 0)
        self.dn[eng] = n + 1
        key = "d%s%d" % (eng, n % self.NDS)
        self._wait(eng, key, self.cnt[key])
        self._deps(eng, reads, writes, key)
        self.cnt[key] += 16
        c = self.cnt[key]
        sem = self.sem[key]
        self.q[eng].append(lambda e, fn=fn, sem=sem: fn(e).then_inc(sem, 16))
        for b in reads:
            b.r[key] = c
        for b in writes:
            b.w = (key, c)
            b.r = {}

    def coll(self, fn, reads=(), writes=()):
        self._deps("pool", reads, writes, "cc")
        self.cnt["cc"] += 1
        c = self.cnt["cc"]
        sem = self.sem["cc"]
        self.q["pool"].append(lambda e, fn=fn, sem=sem: fn(e).then_inc(sem, 1))
        for b in reads:
            b.r["cc"] = c
        for b in writes:
            b.w = ("cc", c)
            b.r = {}

    def barrier(self):
        for eng in self.ENG:
            for k, v in self.cnt.items():
                self._wait(eng, k, v)

    def flush(self, block):
        q = self.q
        self.q = {n: [] for n in self.ENG}

        @block.tensor
        def _(e):
            for f in q["pe"]:
                f(e)

        @block.scalar
        def _(e):
            for f in q["act"]:
                f(e)

        @block.vector
        def _(e):
            for f in q["dve"]:
                f(e)

        @block.gpsimd
        def _(e):
            for f in q["pool"]:
                f(e)

        @block.sync
        def _(e):
            for f in q["sp"]:
                f(e)


def build():
    nc = bass.Bass("TRN2", target_bir_lowering=False)

    def din(name, shape, dt=F32):
        return nc.dram_tensor(name, shape, dt, kind="ExternalInput").ap()

    x_own = din("x_own", [NT, D])
    cT_d = din("cT", [128, 8])
    ada_w = din("ada_w", [D, 9 * D])
    ada_bT = din("ada_bT", [128, 72])
    nT_d = din("nT", [128, 3, 8])
    ffw = {}
    for t in ("f1", "f2"):
        ffw[t] = (din(t + "wg", [D, DFF]), din(t + "wu", [D, DFF]), din(t + "wd", [DFF, D]))
    identf_d = din("identf", [128, 128])
    onesb_d = din("onesb", [128, 128], BF16)
    out_d = nc.dram_tensor("out", [NT, D], F32, kind="ExternalOutput").ap()
    w_in = din("w_in", [D, INW])
    bfT_d = din("bfT", [8, 1])
    gn_d = din("gnT", [128, 4])
    onesbd_d = din("onesbd", [128, 128], BF16)
    w_brA = din("w_brA", [512, D])
    w_brF = din("w_brF", [512, D])
    w_out = din("w_out", [D, D])
    identb_d = din("identb", [128, 128], BF16)
    sel_d = din("sel", [128, 8, 128], BF16)
    cmaskf_d = din("cmaskf", [128, 512])
    cmT_d = din("cmT", [128, 4, 128], BF16)
    posK_d = din("posK", [2, NT], BF16)
    seljF_d = din("seljF", [8, 4])
    pow2_d = din("pow2", [128, NIT])
    posrow_d = din("posrow", [128, 512])
    NKC = 13
    NVC = 8
    XKc = [nc.dram_tensor("XK%d" % i, [128, NT], BF16) for i in range(NKC)]
    AGKc = [nc.dram_tensor("AGK%d" % i, [4 * 128, NT], BF16) for i in range(NKC)]
    XVc = [nc.dram_tensor("XV%d" % i, [128, 2048], BF16) for i in range(NVC)]
    AGVc = [nc.dram_tensor("AGV%d" % i, [4 * 128, 2048], BF16) for i in range(NVC)]
    XL = nc.dram_tensor("XL", [8, NT], F32)
    AGL = nc.dram_tensor("AGL", [4 * 8, NT], F32)
    QAd = nc.dram_tensor("QAd", [8, 64, NT], BF16)
    QFd = nc.dram_tensor("QFd", [8, 64, NT], BF16)
    QId = nc.dram_tensor("QId", [64, 8, NT], BF16)
    Wd = nc.dram_tensor("Wd", [8, NT], F32)
    FKd = nc.dram_tensor("FKd", [8, 3, 4, NT], BF16)
    FQd = nc.dram_tensor("FQd", [8, 3, NT], BF16)
    OTd = nc.dram_tensor("OTd", [16, 64, NT], BF16)
    XTd = nc.dram_tensor("XTd", [128, 8 * NT], F32)
    RG = [[0, 1, 2, 3], [4, 5, 6, 7]]

    with ExitStack() as top:
        E = top.enter_context
        keys = ["pe", "act", "dve", "pool", "sp", "cc"]
        keys += ["dsp%d" % i for i in range(Prog.NDS)] + ["dpool%d" % i for i in range(Prog.NDS)]
        sems = {k: E(nc.semaphore("s_" + k)) for k in keys}
        P = Prog(nc, sems)
        xT_T = [Ts(4) for _ in range(8)]
        modT = E(nc.sbuf_tensor("modT", [128, 72], F32))
        AG = E(nc.sbuf_tensor("AG", [128, 6, 8], F32))
        identf = E(nc.sbuf_tensor("identf_s", [128, 128], F32))
        onesb = E(nc.sbuf_tensor("onesb_s", [128, 128], BF16))
        tMod, tAG, tConst = T(), T(), T()
        xstack = ExitStack()
        xT = xstack.enter_context(nc.sbuf_tensor("xT", [128, 8, NT], F32))

        def tok(tt):
            return slice(tt * 512, (tt + 1) * 512)

        with ExitStack() as ph:
            Ep = ph.enter_context
            cT = Ep(nc.sbuf_tensor("cT_s", [128, 8], F32))
            csil = Ep(nc.sbuf_tensor("csil", [128, 8], BF16))
            abT = Ep(nc.sbuf_tensor("abT", [128, 72], F32))
            nT = Ep(nc.sbuf_tensor("nT_s", [128, 3, 8], F32))
            adaw = [Ep(nc.sbuf_tensor("adaw%d" % i, [128, 8, 1024], BF16)) for i in range(2)]
            xs = [Ep(nc.sbuf_tensor("xs%d" % i, [128, D], F32)) for i in range(2)]
            modps = Ep(nc.psum_tensor("modps", [128, 512], F32))
            tps = [Ep(nc.psum_tensor("tps%d" % i, [128, 512], F32)) for i in range(4)]
            block = Ep(nc.Block())
            t_c, t_cs, t_ab, t_n, t_mps = Ts(5)
            t_adaw, t_xs, t_tps = Ts(2), Ts(2), Ts(4)
            P.dma("sp", lambda e: e.dma_start(out=identf[:], in_=identf_d), writes=[tConst])
            P.dma("sp", lambda e: e.dma_start(out=onesb[:], in_=onesb_d), writes=[tConst])
            P.dma("sp", lambda e: e.dma_start(out=cT[:], in_=cT_d), writes=[t_c])
            P.dma("sp", lambda e: e.dma_start(out=abT[:], in_=ada_bT), writes=[t_ab])
            P.dma("sp", lambda e: e.dma_start(out=nT[:], in_=nT_d), writes=[t_n])
            P.op("act", lambda e: e.activation(out=csil[:], in_=cT[:], func=AF.Silu), reads=[t_c], writes=[t_cs])
            for gi in range(9):
                b = gi % 2
                P.dma("pool", lambda e, gi=gi, b=b: e.dma_start(
                    out=adaw[b][:], in_=ada_w[:, gi * 1024:(gi + 1) * 1024].rearrange("(kc p) f -> p kc f", p=128)),
                    writes=[t_adaw[b]])
                for fc in range(8):
                    col = gi * 8 + fc
                    for kc in range(8):
                        P.op("pe", lambda e, b=b, fc=fc, kc=kc, col=col: e.matmul(
                            modps[:, col:col + 1], adaw[b][:, kc, fc * 128:(fc + 1) * 128], csil[:, kc:kc + 1],
                            start=(kc == 0), stop=(kc == 7)), reads=[t_adaw[b], t_cs], writes=[t_mps])
            P.op("dve", lambda e: e.tensor_tensor(out=modT[:], in0=modps[:, 0:72], in1=abT[:], op=ALU.add),
                 reads=[t_mps, t_ab], writes=[tMod])
            for k in range(3):
                P.op("dve", lambda e, k=k: e.scalar_tensor_tensor(
                    out=AG[:, 2 * k, :], in0=modT[:, (3 * k + 1) * 8:(3 * k + 2) * 8], scalar=1.0, in1=nT[:, k, :],
                    op0=ALU.add, op1=ALU.mult), reads=[tMod, t_n], writes=[tAG])
                gsc = 1.0 if k == 1 else 0.5
                P.op("dve", lambda e, k=k, gsc=gsc: e.tensor_scalar(
                    out=AG[:, 2 * k + 1, :], in0=modT[:, (3 * k + 2) * 8:(3 * k + 3) * 8], scalar1=gsc, scalar2=None,
                    op0=ALU.mult), reads=[tMod], writes=[tAG])
            ev = 0
            for m in range(16):
                b = m % 2
                P.dma("sp", lambda e, m=m, b=b: e.dma_start(out=xs[b][:], in_=x_own[m * 128:(m + 1) * 128, :]),
                      writes=[t_xs[b]])
                for half in range(2):
                    pb = (2 * m + half) % 4
                    for q in range(4):
                        fc = half * 4 + q
                        P.op("pe", lambda e, b=b, pb=pb, q=q, fc=fc: e.transpose(
                            tps[pb][:, q * 128:(q + 1) * 128], xs[b][:, fc * 128:(fc + 1) * 128], identf[:]),
                            reads=[t_xs[b], tConst], writes=[t_tps[pb]])
                    wr = [xT_T[fc][m // 4] for fc in range(half * 4, half * 4 + 4)]
                    src = lambda pb=pb: tps[pb][:, :].rearrange("p (q t) -> p q t", q=4)
                    dst = lambda half=half, m=m: xT[:, half * 4:(half + 1) * 4, m * 128:(m + 1) * 128]
                    if ev % 2 == 0:
                        P.op("dve", lambda e, src=src, dst=dst: e.tensor_copy(out=dst(), in_=src()),
                             reads=[t_tps[pb]], writes=wr)
                    else:
                        P.op("act", lambda e, src=src, dst=dst: e.activation(out=dst(), in_=src(), func=AF.Copy),
                             reads=[t_tps[pb]], writes=wr)
                    ev += 1
            P.barrier()
            P.flush(block)

        def ffn(tag, k):
            wg, wu, wd = ffw[tag]
            with ExitStack() as ph:
                Ep = ph.enter_context
                hT = Ep(nc.sbuf_tensor(tag + "hT", [128, 8, NT], BF16))
                actT = Ep(nc.sbuf_tensor(tag + "actT", [128, 8, NT], BF16))
                sq = Ep(nc.sbuf_tensor(tag + "sq", [128, 8, 512], BF16))
                rt = Ep(nc.sbuf_tensor(tag + "rt", [128, 512], F32))
                rstd = Ep(nc.sbuf_tensor(tag + "rstd", [128, 512], F32))
                tmp = [Ep(nc.sbuf_tensor(tag + "tmp%d" % i, [128, 512], F32)) for i in range(2)]
                sgb = [Ep(nc.sbuf_tensor(tag + "sg%d" % i, [128, 512], F32)) for i in range(2)]
                wgb = [Ep(nc.sbuf_tensor(tag + "wg%d" % i, [128, 8, 128], BF16)) for i in range(2)]
                wub = [Ep(nc.sbuf_tensor(tag + "wu%d" % i, [128, 8, 128], BF16)) for i in range(2)]
                wdb = [Ep(nc.sbuf_tensor(tag + "wd%d" % i, [128, 8, 128], BF16)) for i in range(2)]
                pst = Ep(nc.psum_tensor(tag + "pst", [128, 512], F32))
                psg = [Ep(nc.psum_tensor(tag + "psg%d" % i, [128, 512], F32)) for i in range(2)]
                psu = [Ep(nc.psum_tensor(tag + "psu%d" % i, [128, 512], F32)) for i in range(2)]
                psd = [Ep(nc.psum_tensor(tag + "psd%d" % i, [128, 512], F32)) for i in range(2)]
                block = Ep(nc.Block())
                t_hT = [Ts(4) for _ in range(8)]
                t_act = [Ts(4) for _ in range(8)]
                t_sq, t_rt, t_rstd, t_pst = Ts(4)
                t_tmp, t_sg, t_wg, t_wu, t_wd, t_psg, t_psu, t_psd = (Ts(2) for _ in range(8))
                Acol = lambda kc: AG[:, 2 * k, kc:kc + 1]
                Gcol = lambda kc: AG[:, 2 * k + 1, kc:kc + 1]
                shcol = lambda kc: modT[:, 3 * k * 8 + kc:3 * k * 8 + kc + 1]
                for tt in range(4):
                    P.op("act", lambda e, tt=tt: e.activation(out=sq[:], in_=xT[:, :, tok(tt)], func=AF.Square),
                         reads=[xT_T[fc][tt] for fc in range(8)], writes=[t_sq])
                    for kc in range(8):
                        P.op("pe", lambda e, kc=kc: e.matmul(pst[:], onesb[:], sq[:, kc, :], start=(kc == 0), stop=(kc == 7)),
                             reads=[t_sq, tConst], writes=[t_pst])
                    P.op("act", lambda e: e.activation(out=rt[:], in_=pst[:], func=AF.Sqrt, scale=1.0 / D, bias=EPS),
                         reads=[t_pst], writes=[t_rt])
                    P.op("dve", lambda e: e.reciprocal(out=rstd[:], in_=rt[:]), reads=[t_rt], writes=[t_rstd])
                    for kc in range(8):
                        b = kc % 2
                        P.op("dve", lambda e, kc=kc, b=b, tt=tt: e.scalar_tensor_tensor(
                            out=tmp[b][:], in0=xT[:, kc, tok(tt)], scalar=Acol(kc), in1=rstd[:], op0=ALU.mult, op1=ALU.mult),
                            reads=[xT_T[kc][tt], t_rstd, tAG], writes=[t_tmp[b]])
                        P.op("act", lambda e, kc=kc, b=b, tt=tt: e.activation(
                            out=hT[:, kc, tok(tt)], in_=tmp[b][:], func=AF.Identity, bias=shcol(kc)),
                            reads=[t_tmp[b], tMod], writes=[t_hT[kc][tt]])
                groups = [list(range(0, 8)), list(range(8, 15)), list(range(15, 22))]
                wi = 0
                di = 0
                pi = 0
                for js in groups:
                    for jj, j in enumerate(js):
                        b = wi % 2
                        wi += 1
                        P.dma("pool", lambda e, b=b, j=j: e.dma_start(
                            out=wgb[b][:], in_=wg[:, j * 128:(j + 1) * 128].rearrange("(kc p) f -> p kc f", p=128)),
                            writes=[t_wg[b]])
                        P.dma("pool", lambda e, b=b, j=j: e.dma_start(
                            out=wub[b][:], in_=wu[:, j * 128:(j + 1) * 128].rearrange("(kc p) f -> p kc f", p=128)),
                            writes=[t_wu[b]])
                        for tt in range(4):
                            pb = pi % 2
                            pi += 1
                            for kc in range(8):
                                P.op("pe", lambda e, b=b, pb=pb, kc=kc, tt=tt: e.matmul(
                                    psg[pb][:], wgb[b][:, kc, :], hT[:, kc, tok(tt)], start=(kc == 0), stop=(kc == 7)),
                                    reads=[t_wg[b], t_hT[kc][tt]], writes=[t_psg[pb]])
                            for kc in range(8):
                                P.op("pe", lambda e, b=b, pb=pb, kc=kc, tt=tt: e.matmul(
                                    psu[pb][:], wub[b][:, kc, :], hT[:, kc, tok(tt)], start=(kc == 0), stop=(kc == 7)),
                                    reads=[t_wu[b], t_hT[kc][tt]], writes=[t_psu[pb]])
                            P.op("act", lambda e, pb=pb: e.activation(out=sgb[pb][:], in_=psg[pb][:], func=AF.Silu),
                                 reads=[t_psg[pb]], writes=[t_sg[pb]])
                            P.op("dve", lambda e, pb=pb, jj=jj, tt=tt: e.tensor_tensor(
                                out=actT[:, jj, tok(tt)], in0=sgb[pb][:], in1=psu[pb][:], op=ALU.mult),
                                reads=[t_sg[pb], t_psu[pb]], writes=[t_act[jj][tt]])
                    n = len(js)
                    j0 = js[0]
                    for d in range(8):
                        b = di % 2
                        di += 1
                        P.dma("pool", lambda e, b=b, d=d, n=n, j0=j0: e.dma_start(
                            out=wdb[b][:, 0:n, :],
                            in_=wd[j0 * 128:(j0 + n) * 128, d * 128:(d + 1) * 128].rearrange("(jj p) f -> p jj f", p=128)),
                            writes=[t_wd[b]])
                        for tt in range(4):
                            pb = (d * 4 + tt) % 2
                            for jj in range(n):
                                P.op("pe", lambda e, b=b, pb=pb, jj=jj, tt=tt, n=n: e.matmul(
                                    psd[pb][:], wdb[b][:, jj, :], actT[:, jj, tok(tt)], start=(jj == 0), stop=(jj == n - 1)),
                                    reads=[t_wd[b], t_act[jj][tt]], writes=[t_psd[pb]])
                            P.op("dve", lambda e, pb=pb, d=d, tt=tt: e.scalar_tensor_tensor(
                                out=xT[:, d, tok(tt)], in0=psd[pb][:], scalar=Gcol(d), in1=xT[:, d, tok(tt)],
                                op0=ALU.mult, op1=ALU.add),
                                reads=[t_psd[pb], tAG], writes=[xT_T[d][tt]])
                P.barrier()
                P.flush(block)

        if STAGES >= 1:
            ffn("f1", 0)

        def MM(o, l, r, st, sp_, rd, wr):
            P.op("pe", lambda e: e.matmul(o, l, r, start=st, stop=sp_), rd, wr)

        def TR(o, i, idn, rd, wr):
            P.op("pe", lambda e: e.transpose(o, i, idn), rd, wr)

        def ACTV(o, i, f, rd, wr, scale=None, bias=None):
            kw = {}
            if scale is not None:
                kw["scale"] = scale
            if bias is not None:
                kw["bias"] = bias
            P.op("act", lambda e: e.activation(out=o, in_=i, func=f, **kw), rd, wr)

        def TS(o, i, s1, s2, op0, op1, rd, wr, acc=None):
            kw = {}
            if op1 is not None:
                kw["op1"] = op1
            if acc is not None:
                kw["accum_out"] = acc
            P.op("dve", lambda e: e.tensor_scalar(out=o, in0=i, scalar1=s1, scalar2=s2, op0=op0, **kw), rd, wr)

        def TT(o, a, b, op, rd, wr):
            P.op("dve", lambda e: e.tensor_tensor(out=o, in0=a, in1=b, op=op), rd, wr)

        def STT(o, a, s, b, op0, op1, rd, wr):
            P.op("dve", lambda e: e.scalar_tensor_tensor(out=o, in0=a, scalar=s, in1=b, op0=op0, op1=op1), rd, wr)

        def TC(o, i, rd, wr):
            P.op("dve", lambda e: e.tensor_copy(out=o, in_=i), rd, wr)

        def RED(o, i, op, rd, wr):
            P.op("dve", lambda e: e.tensor_reduce(out=o, in_=i, axis=AX.X, op=op), rd, wr)

        def RCP(o, i, rd, wr):
            P.op("dve", lambda e: e.reciprocal(out=o, in_=i), rd, wr)

        def MSET(o, v, wr):
            P.op("dve", lambda e: e.memset(o, v), (), wr)

        def DMA(o, i, rd, wr, q="sp", **kw):
            P.dma(q, lambda e: e.dma_start(out=o, in_=i, **kw), rd, wr)

        evc = [0]

        def COPY(o, i, rd, wr):
            if evc[0] % 2 == 0:
                TC(o, i, rd, wr)
            else:
                ACTV(o, i, AF.Copy, rd, wr)
            evc[0] += 1

        def emit_norm(k, tt, nb, dst, t_dst):
            sq, rt, rstd, tmp, pst, t_sq, t_rt, t_rstd, t_tmp, t_pst = nb
            ACTV(sq[:], xT[:, :, tok(tt)], AF.Square, [xT_T[fc][tt] for fc in range(8)], [t_sq])
            for kc in range(8):
                MM(pst[:], onesb[:], sq[:, kc, :], kc == 0, kc == 7, [t_sq, tConst], [t_pst])
            ACTV(rt[:], pst[:], AF.Sqrt, [t_pst], [t_rt], scale=1.0 / D, bias=EPS)
            RCP(rstd[:], rt[:], [t_rt], [t_rstd])
            for kc in range(8):
                b = kc % 2
                STT(tmp[b][:], xT[:, kc, tok(tt)], AG[:, 2 * k, kc:kc + 1], rstd[:], ALU.mult, ALU.mult,
                    [xT_T[kc][tt], t_rstd, tAG], [t_tmp[b]])
                ACTV(dst(kc), tmp[b][:], AF.Identity, [t_tmp[b], tMod], [t_dst(kc)],
                     bias=modT[:, 3 * k * 8 + kc:3 * k * 8 + kc + 1])

        def norm_bufs(Ep, tag):
            sq = Ep(nc.sbuf_tensor(tag + "sq", [128, 8, 512], BF16))
            rt = Ep(nc.sbuf_tensor(tag + "rt", [128, 512], F32))
            rstd = Ep(nc.sbuf_tensor(tag + "rstd", [128, 512], F32))
            tmp = [Ep(nc.sbuf_tensor(tag + "tmp%d" % i, [128, 512], F32)) for i in range(2)]
            pst = Ep(nc.psum_tensor(tag + "pst", [128, 512], F32))
            return (sq, rt, rstd, tmp, pst, T(), T(), T(), Ts(2), T())

        tXK, tAGK, tXV, tAGV = Ts(NKC), Ts(NKC), Ts(NVC), Ts(NVC)
        tXL, tAGL = Ts(2)
        tQAd, tQFd, tQId, tWd, tFKd, tFQd, tOTd, tXTd = Ts(8)

        def phase3():
            with ExitStack() as ph:
                Ep = ph.enter_context
                h2T = Ep(nc.sbuf_tensor("p3h2T", [128, 8, NT], BF16))
                nb = norm_bufs(Ep, "p3")
                wt = [Ep(nc.sbuf_tensor("p3wt%d" % i, [128, 8, 512], BF16)) for i in range(2)]
                gn = Ep(nc.sbuf_tensor("p3gn", [128, 4], F32))
                gq = Ep(nc.sbuf_tensor("p3gq", [128, 4], F32))
                onesbd = Ep(nc.sbuf_tensor("p3onesbd", [128, 128], BF16))
                bfT = Ep(nc.sbuf_tensor("p3bf", [8, 1], F32))
                nbf = Ep(nc.sbuf_tensor("p3nbf", [8, 1], F32))
                sqh = [Ep(nc.sbuf_tensor("p3sqh%d" % i, [128, 512], BF16)) for i in range(2)]
                rth = [Ep(nc.sbuf_tensor("p3rth%d" % i, [128, 512], F32)) for i in range(2)]
                rsh = [Ep(nc.sbuf_tensor("p3rsh%d" % i, [128, 512], F32)) for i in range(2)]
                nzs = [Ep(nc.sbuf_tensor("p3nzs%d" % i, [128, NT], BF16)) for i in range(2)]
                vbig = [Ep(nc.sbuf_tensor("p3vbig%d" % i, [128, 8, 16, 64], BF16)) for i in range(2)]
                wst = Ep(nc.sbuf_tensor("p3wst", [8, NT], F32))
                lst = Ep(nc.sbuf_tensor("p3lst", [8, NT], F32))
                let_ = Ep(nc.sbuf_tensor("p3let", [8, 512], F32))
                psz = [Ep(nc.psum_tensor("p3psz%d" % i, [128, 512], F32)) for i in range(2)]
                pss = [Ep(nc.psum_tensor("p3pss%d" % i, [128, 512], F32)) for i in range(2)]
                psv = [Ep(nc.psum_tensor("p3psv%d" % i, [128, 512], F32)) for i in range(2)]
                block = Ep(nc.Block())
                t_h2 = [Ts(4) for _ in range(8)]
                t_wt, t_sqh, t_rth, t_rsh, t_nzs, t_vst, t_psz, t_pss, t_psv = (Ts(2) for _ in range(9))
                t_gn, t_gq, t_bf, t_nbf, t_wst, t_lst, t_let = Ts(7)
                DMA(gn[:], gn_d, [], [t_gn])
                DMA(onesbd[:], onesbd_d, [], [t_gn])
                DMA(bfT[:], bfT_d, [], [t_bf])
                TS(gq[:], gn[:], 0.125, None, ALU.mult, None, [t_gn], [t_gq])
                TS(nbf[:], bfT[:], -1.0, None, ALU.mult, None, [t_bf], [t_nbf])
                for h in range(8):
                    DMA(XKc[h].ap()[64:66, :], posK_d, [], [tXK[h]])
                for tt in range(4):
                    emit_norm(1, tt, nb, lambda kc, tt=tt: h2T[:, kc, tok(tt)], lambda kc, tt=tt: t_h2[kc][tt])
                cnt = {"w": 0, "p": 0, "s": 0, "v": 0}

                wgroups = [(3584, 80), (512, 512), (2048, 512), (1024, 512), (2560, 512), (0, 512), (1536, 512), (3072, 512)]
                wstate = {"next": 0}

                def issue_w():
                    i = wstate["next"]
                    if i < len(wgroups):
                        c0_, nc_ = wgroups[i]
                        DMA(wt[i % 2][:, :, 0:nc_], w_in[:, c0_:c0_ + nc_].rearrange("(kc p) f -> p kc f", p=128), [], [t_wt[i % 2]], q="pool")
                        wstate["next"] = i + 1

                def load_w(c0, ncols):
                    i = cnt["w"]
                    assert wgroups[i] == (c0, ncols)
                    cnt["w"] += 1
                    if wstate["next"] <= i:
                        issue_w()
                    issue_w()
                    return i % 2

                def gather(ci_list, kind):
                    for ci in ci_list:
                        if kind == "k":
                            src, dst, t_s, t_d = XKc[ci], AGKc[ci], tXK[ci], tAGK[ci]
                        elif kind == "v":
                            src, dst, t_s, t_d = XVc[ci], AGVc[ci], tXV[ci], tAGV[ci]
                        else:
                            src, dst, t_s, t_d = XL, AGL, tXL, tAGL
                        P.coll(lambda e, src=src, dst=dst: e.collective_compute(
                            "AllGather", ALU.bypass, replica_groups=RG, ins=[src.ap().opt()], outs=[dst.ap().opt()]),
                            reads=[t_s], writes=[t_d])

                def proj(b, off, M, tt):
                    pb = cnt["p"] % 2
                    cnt["p"] += 1
                    for kc in range(8):
                        MM(psz[pb][0:M, :], wt[b][:, kc, off:off + M], h2T[:, kc, tok(tt)], kc == 0, kc == 7,
                           [t_wt[b], t_h2[kc][tt]], [t_psz[pb]])
                    return pb

                def qk_group(c0, gcol, t_g, dst_fn, t_dst, after=None):
                    b = load_w(c0, 512)
                    for hp in range(4):
                        sb = cnt["s"] % 2
                        cnt["s"] += 1
                        for tt in range(4):
                            pb = proj(b, hp * 128, 128, tt)
                            ACTV(sqh[pb][:], psz[pb][:], AF.Square, [t_psz[pb]], [t_sqh[pb]])
                            MM(pss[pb][:], onesbd[:], sqh[pb][:], True, True, [t_sqh[pb], t_gn], [t_pss[pb]])
                            ACTV(rth[pb][:], pss[pb][:], AF.Sqrt, [t_pss[pb]], [t_rth[pb]], scale=1.0 / 64, bias=EPS)
                            RCP(rsh[pb][:], rth[pb][:], [t_rth[pb]], [t_rsh[pb]])
                            STT(nzs[sb][:, tok(tt)], psz[pb][:], gcol, rsh[pb][:], ALU.mult, ALU.mult,
                                [t_psz[pb], t_rsh[pb], t_g], [t_nzs[sb]])
                        for hl in range(2):
                            h = 2 * hp + hl
                            DMA(dst_fn(h), nzs[sb][hl * 64:(hl + 1) * 64, :], [t_nzs[sb]], [t_dst(h)])
                            if after is not None:
                                after(h)

                b = load_w(3584, 80)
                sb = cnt["s"] % 2
                cnt["s"] += 1
                for tt in range(4):
                    pb = proj(b, 0, 64, tt)
                    COPY(nzs[sb][0:64, tok(tt)], psz[pb][0:64, :], [t_psz[pb]], [t_nzs[sb]])
                DMA(XKc[12].ap()[0:64, :], nzs[sb][0:64, :], [t_nzs[sb]], [tXK[12]])
                gather([12], "k")
                for tt in range(4):
                    pb = proj(b, 64, 8, tt)
                    COPY(wst[:, tok(tt)], psz[pb][0:8, :], [t_psz[pb]], [t_wst])
                DMA(Wd.ap(), wst[:], [t_wst], [tWd])
                for tt in range(4):
                    pb = proj(b, 72, 8, tt)
                    ACTV(let_[:], psz[pb][0:8, :], AF.Exp, [t_psz[pb], t_nbf], [t_let], scale=-1.0, bias=nbf[:, 0:1])
                    ACTV(let_[:], let_[:], AF.Ln, [t_let], [t_let], scale=1.0, bias=1.0)
                    TS(lst[:, tok(tt)], let_[:], -1.0, None, ALU.mult, None, [t_let], [t_lst])
                DMA(XL.ap(), lst[:], [t_lst], [tXL])
                gather([0], "l")
                qk_group(512, gn[:, 1:2], t_gn, lambda h: XKc[h].ap()[0:64, :], lambda h: tXK[h],
                         after=lambda h: gather([h], "k"))
                qk_group(2048, gn[:, 3:4], t_gn, lambda h: XKc[8 + h // 2].ap()[(h % 2) * 64:(h % 2) * 64 + 64, :],
                         lambda h: tXK[8 + h // 2], after=lambda h: (gather([8 + h // 2], "k") if h % 2 == 1 else None))

                XV4 = [XVc[i].ap().rearrange("p (hh m d) -> p hh m d", hh=2, m=16) for i in range(NVC)]
                for vi, c0 in enumerate((1024, 2560)):
                    b = load_w(c0, 512)
                    for mt in range(16):
                        pb = cnt["v"] % 2
                        cnt["v"] += 1
                        for kc in range(8):
                            MM(psv[pb][:], h2T[:, kc, mt * 128:(mt + 1) * 128], wt[b][:, kc, :], kc == 0, kc == 7,
                               [t_wt[b], t_h2[kc][mt // 4]], [t_psv[pb]])
                        COPY(vbig[vi][:, :, mt, :], psv[pb][:, :].rearrange("p (h d) -> p h d", h=8), [t_psv[pb]], [t_vst[vi]])
                    for cc in range(4):
                        DMA(XVc[vi * 4 + cc].ap(), vbig[vi][:, 2 * cc:2 * cc + 2, :, :].rearrange("p h m d -> p (h m d)"),
                            [t_vst[vi]], [tXV[vi * 4 + cc]])
                    gather([vi * 4 + cc for cc in range(4)], "v")

                qk_group(0, gq[:, 0:1], t_gq, lambda h: QAd.ap()[h], lambda h: tQAd)
                qk_group(1536, gq[:, 2:3], t_gq, lambda h: QFd.ap()[h], lambda h: tQFd)
                b = load_w(3072, 512)
                for ip in range(4):
                    sb = cnt["s"] % 2
                    cnt["s"] += 1
                    for tt in range(4):
                        pb = proj(b, ip * 128, 128, tt)
                        COPY(nzs[sb][:, tok(tt)], psz[pb][:], [t_psz[pb]], [t_nzs[sb]])
                    for il in range(2):
                        DMA(QId.ap()[:, 2 * ip + il, :], nzs[sb][il * 64:(il + 1) * 64, :], [t_nzs[sb]], [tQId])
                DMA(XTd.ap(), xT[:, :, :].rearrange("p a t -> p (a t)"),
                    [xT_T[fc][tt] for fc in range(8) for tt in range(4)], [tXTd])
                P.barrier()
                P.flush(block)

        def phase35():
            with ExitStack() as ph:
                Ep = ph.enter_context
                W = 4096
                lf = Ep(nc.sbuf_tensor("p35lf", [8, W], F32))
                Fc = Ep(nc.sbuf_tensor("p35F", [8, W], F32))
                Fo = Ep(nc.sbuf_tensor("p35Fo", [8, 1024], F32))
                ones = Ep(nc.sbuf_tensor("p35ones", [8, W], F32))
                carry = Ep(nc.sbuf_tensor("p35carry", [8, 1], F32))
                selj = Ep(nc.sbuf_tensor("p35selj", [8, 4], F32))
                r1 = Ep(nc.sbuf_tensor("p35r1", [8, W], F32))
                hb = [Ep(nc.sbuf_tensor("p35hb%d" % i, [8, W], BF16)) for i in range(3)]
                nhb = [Ep(nc.sbuf_tensor("p35nhb%d" % i, [8, W], BF16)) for i in range(3)]
                r1o = Ep(nc.sbuf_tensor("p35r1o", [8, 1024], F32))
                hbo = [Ep(nc.sbuf_tensor("p35hbo%d" % i, [8, 1024], BF16)) for i in range(3)]
                block = Ep(nc.Block())
                t_lf, t_F, t_Fo, t_ones, t_carry, t_selj, t_r1, t_r1o = Ts(8)
                t_hb, t_nhb, t_hbo = Ts(3), Ts(3), Ts(3)
                MSET(ones[:], 1.0, [t_ones])
                DMA(selj[:], seljF_d, [], [t_selj])

                def split(src, t_src, n, r, t_r, pieces, t_pieces):
                    TC(pieces[0][:, 0:n], src[:, 0:n], [t_src], [t_pieces[0]])
                    TT(r[:, 0:n], src[:, 0:n], pieces[0][:, 0:n], ALU.subtract, [t_src, t_pieces[0]], [t_r])
                    TC(pieces[1][:, 0:n], r[:, 0:n], [t_r], [t_pieces[1]])
                    TT(src[:, 0:n], r[:, 0:n], pieces[1][:, 0:n], ALU.subtract, [t_r, t_pieces[1]], [t_src])
                    TC(pieces[2][:, 0:n], src[:, 0:n], [t_src], [t_pieces[2]])

                lf4 = lf[:, :].rearrange("h (m j p) -> h m j p", m=8, j=4)
                F4 = Fc[:, :].rearrange("h (m j p) -> h m j p", m=8, j=4)
                Fo3 = Fo[:, :].rearrange("h (m p) -> h m p", m=8)
                for q in range(2):
                    for j in range(4):
                        DMA(lf4[:, :, j, :], AGL.ap()[j * 8:(j + 1) * 8, q * 1024:(q + 1) * 1024].rearrange("h (m p) -> h m p", p=128),
                            [tAGL], [t_lf])
                    init = 0.0 if q == 0 else carry[:, 0:1]
                    P.op("dve", lambda e, init=init: e.tensor_tensor_scan(out=Fc[:], data0=ones[:], data1=lf[:], initial=init,
                                                                          op0=ALU.mult, op1=ALU.add),
                         reads=[t_ones, t_lf, t_carry], writes=[t_F])
                    TC(carry[:], Fc[:, W - 1:W], [t_F], [t_carry])
                    TS(Fo3, F4[:, :, 0, :], selj[:, 0:1], None, ALU.mult, None, [t_F, t_selj], [t_Fo])
                    for j in range(1, 4):
                        STT(Fo3, F4[:, :, j, :], selj[:, j:j + 1], Fo3, ALU.mult, ALU.add, [t_F, t_selj, t_Fo], [t_Fo])
                    split(Fo, t_Fo, 1024, r1o, t_r1o, hbo, t_hbo)
                    for r in range(3):
                        DMA(FQd.ap()[:, r, q * 1024:(q + 1) * 1024], hbo[r][:], [t_hbo[r]], [tFQd])
                    split(Fc, t_F, W, r1, t_r1, hb, t_hb)
                    for r in range(3):
                        TS(nhb[r][:], hb[r][:], -1.0, None, ALU.mult, None, [t_hb[r]], [t_nhb[r]])
                        nh4 = nhb[r][:, :].rearrange("h (m j p) -> h m j p", m=8, j=4)
                        for j in range(4):
                            DMA(FKd.ap()[:, r, j, q * 1024:(q + 1) * 1024].rearrange("h (m p) -> h m p", p=128),
                                nh4[:, :, j, :], [t_nhb[r]], [tFKd])
                P.barrier()
                P.flush(block)

        def phase4():
            FP8 = mybir.dt.float8e4
            with ExitStack() as ph:
                Ep = ph.enter_context
                scores2 = [Ep(nc.sbuf_tensor("p4sc%d" % i, [128, S], F32)) for i in range(2)]
                maskb1 = Ep(nc.sbuf_tensor("p4mb", [128, S], BF16))
                maskb2 = [maskb1, maskb1]
                cntj = Ep(nc.sbuf_tensor("p4cntj", [128, S], FP8))
                maskT = Ep(nc.sbuf_tensor("p4mT", [128, 64, 512], FP8))
                kidx = Ep(nc.sbuf_tensor("p4kidx", [128, 8, 512], BF16))
                QIb1 = Ep(nc.sbuf_tensor("p4qi", [128, 8, 128], BF16))
                QIb = [QIb1, QIb1]
                QIg = [Ep(nc.sbuf_tensor("p4qg%d" % i, [128, 8, 128], BF16)) for i in range(2)]
                QAt = Ep(nc.sbuf_tensor("p4qa", [68, 8, 512], BF16))
                QFt = Ep(nc.sbuf_tensor("p4qf", [70, 8, 512], BF16))
                NKB = 2
                NPT = 4
                LAG = 3
                NVB = 4
                KAb = [Ep(nc.sbuf_tensor("p4ka%d" % i, [68, 4, 512], BF16)) for i in range(NKB)]
                KFb = [Ep(nc.sbuf_tensor("p4kf%d" % i, [70, 4, 512], BF16)) for i in range(NKB)]
                Vb = [Ep(nc.sbuf_tensor("p4v%d" % i, [128, 4, 4, 65], BF16)) for i in range(NVB)]
                Rsb = [Ep(nc.sbuf_tensor("p4rs%d" % i, [128, 512], BF16)) for i in range(3)]
                PT = [Ep(nc.sbuf_tensor("p4pt%d" % i, [128, 512], BF16)) for i in range(NPT)]
                on = [Ep(nc.sbuf_tensor("p4on%d" % i, [65, 512], F32)) for i in range(2)]
                rc = Ep(nc.sbuf_tensor("p4rc", [65, 512], F32))
                onesf = Ep(nc.sbuf_tensor("p4onesf", [65, 64], F32))
                OTs = [Ep(nc.sbuf_tensor("p4ots%d" % i, [64, 512], BF16)) for i in range(2)]
                wcol = Ep(nc.sbuf_tensor("p4wcol", [128, 16, 8], F32))
                sel = Ep(nc.sbuf_tensor("p4sel", [128, 8, 128], BF16))
                identb = Ep(nc.sbuf_tensor("p4idb", [128, 128], BF16))
                identS = Ep(nc.sbuf_tensor("p4idS", [128, 128], BF16))
                cmaskf = Ep(nc.sbuf_tensor("p4cmf", [128, 512], F32))
                cmT = Ep(nc.sbuf_tensor("p4cmT", [128, 4, 128], BF16))
                posrow = Ep(nc.sbuf_tensor("p4pos", [128, 512], F32))
                pow2 = Ep(nc.sbuf_tensor("p4pow2", [128, NIT], F32))
                junk = Ep(nc.sbuf_tensor("p4junk", [128, 512], F32))
                bs = Ep(nc.sbuf_tensor("p4bs", [128, 16], F32))
                Hh = Ep(nc.sbuf_tensor("p4H", [128, NIT], F32))
                U = Ep(nc.sbuf_tensor("p4U", [128, 4], F32))
                sbf = Ep(nc.sbuf_tensor("p4sbf", [128, 2], BF16))
                bank = [Ep(nc.psum_tensor("p4bank%d" % i, [128, 512], F32)) for i in range(7)]
                tpb = Ep(nc.psum_tensor("p4tpb", [128, 1024], BF16))
                t_bank = Ts(7)
                t_tpb = T()
                Rps, t_Rps = bank[0:2], t_bank[0:2]
                scps, t_scps = bank[2:4], t_bank[2:4]
                sps, t_sps = bank[0:4], t_bank[0:4]
                ops, t_ops = bank[4:6], t_bank[4:6]
                bcps, t_bcps = bank[6], t_bank[6]
                block = Ep(nc.Block())
                t_sc2 = [Ts(16), Ts(16)]
                t_mb1 = T()
                t_mb2 = [t_mb1, t_mb1]
                t_cntj = T()
                t_mT = T()
                t_kidx, t_wcol, t_cst, t_bs, t_H, t_U, t_sbf, t_junk, t_rc, t_onesf = Ts(10)
                t_QI1 = T()
                t_QI, t_QG, t_on, t_OTs = [t_QI1, t_QI1], Ts(2), Ts(2), Ts(2)
                t_QA, t_QF = T(), T()
                t_KA, t_KF, t_V = Ts(NKB), Ts(NKB), Ts(NVB)
                t_Rsb, t_PT = Ts(3), Ts(NPT)
                AGK3 = [AGKc[i].ap().rearrange("(j r) c -> j r c", j=4) for i in range(NKC)]
                AGV5 = [AGVc[i].ap().rearrange("(j p) (hh m d) -> j p hh m d", j=4, hh=2, m=16) for i in range(NVC)]
                for (dst, src) in ((sel, sel_d), (identb, identb_d), (cmaskf, cmaskf_d), (cmT, cmT_d),
                                   (posrow, posrow_d), (pow2, pow2_d)):
                    DMA(dst[:], src, [], [t_cst])
                for j in range(4):
                    for par in range(2):
                        DMA(kidx[par * 64:(par + 1) * 64, :, j * 128:(j + 1) * 128],
                            AGK3[12][j, 0:64, :].rearrange("r (s two p) -> r s two p", two=2, p=128)[:, :, par, :], [tAGK[12]], [t_kidx])
                for i in range(8):
                    DMA(wcol[i * 16:(i + 1) * 16, :, :], Wd.ap()[i, :].rearrange("(m g t) -> t m g", g=8, t=16),
                        [tWd], [t_wcol], allow_slow_non_contiguous=True)
                for i in range(NKB):
                    MSET(KAb[i][64:68, :, :], 1.0, [t_KA[i]])
                    MSET(KFb[i][64:70, :, :], 1.0, [t_KF[i]])
                for i in range(NVB):
                    MSET(Vb[i][:, :, :, :], 1.0, [t_V[i]])
                MSET(QFt[64:70, :, :], 1.0, [t_QF])
                MSET(U[:], 1.0, [t_U])
                MSET(onesf[:], 1.0, [t_onesf])
                t_idS = T()
                wabs = Ep(nc.sbuf_tensor("p4wabs", [128, 16, 8], F32))
                wsgn = Ep(nc.sbuf_tensor("p4wsgn", [128, 16, 8], F32))
                Sels = [Ep(nc.sbuf_tensor("p4sels%d" % i, [128, 8, 128], BF16)) for i in range(2)]
                t_wabs, t_wsgn = T(), T()
                t_Sels = Ts(2)
                TS(wsgn[:], wcol[:], 0.0, 2.0, ALU.is_ge, ALU.mult, [t_wcol], [t_wsgn])
                TS(wsgn[:], wsgn[:], -1.0, None, ALU.add, None, [t_wsgn], [t_wsgn])
                TT(wabs[:], wcol[:], wsgn[:], ALU.mult, [t_wcol, t_wsgn], [t_wabs])
                TS(identS[:], identb[:], 128.0, None, ALU.mult, None, [t_cst], [t_idS])
                cn = {"k": 0, "v": 0, "r": 0, "pt": 0, "sp": 0, "op": 0, "sc": 0, "on": 0}

                def load_qbatch(m0):
                    tsl = slice(m0 * 128, m0 * 128 + 512)
                    DMA(QAt[0:64, :, :], QAd.ap()[:, :, tsl].rearrange("h d t -> d h t"), [tQAd], [t_QA])
                    DMA(QFt[0:64, :, :], QFd.ap()[:, :, tsl].rearrange("h d t -> d h t"), [tQFd], [t_QF])
                    DMA(QFt[67:70, :, :], FQd.ap()[:, :, tsl].rearrange("h r t -> r h t"), [tFQd], [t_QF])

                def load_qi(m):
                    qb = m % 2
                    for par in range(2):
                        DMA(QIb[qb][par * 64:(par + 1) * 64, :, :], QId.ap()[:, :, m * 128:(m + 1) * 128], [tQId], [t_QI[qb]])
                    qsrc = QIb[qb][:, :, :].rearrange("d i (g t) -> d g i t", t=16)
                    qdst = QIg[qb][:, :, :].rearrange("d g (i t) -> d g i t", t=16)
                    P.op("pool", lambda e: e.tensor_copy(out=qdst, in_=qsrc), [t_QI[qb]], [t_QG[qb]])
                    for g in range(8):
                        so, si, ss = Sels[qb][:, g, :], sel[:, g, :], wsgn[:, m, g:g + 1]
                        P.op("pool", lambda e, so=so, si=si, ss=ss: e.tensor_scalar(out=so, in0=si, scalar1=ss, scalar2=None, op0=ALU.mult),
                             [t_cst, t_wsgn], [t_Sels[qb]])

                def idx(m, sp):
                    qb = m % 2
                    scores, t_sc = scores2[sp], t_sc2[sp]
                    for mp in range(m + 1):
                        sc = cn["sc"] % 2
                        cn["sc"] += 1
                        p0 = (mp % 2) * 64
                        rhs_k = kidx[p0:p0 + 64, mp // 2, :]

                        def emit_R(g, p0=p0, rhs_k=rhs_k):
                            MM(Rps[g % 2][:], QIg[qb][p0:p0 + 64, g, :], rhs_k, True, True, [t_QG[qb], t_kidx], [t_Rps[g % 2]])
                        emit_R(0)
                        for g in range(8):
                            rb = g % 2
                            rs = cn["r"] % 3
                            cn["r"] += 1
                            ACTV(Rsb[rs][:], Rps[rb][:], AF.Relu, [t_Rps[rb], t_wabs], [t_Rsb[rs]], scale=wabs[:, m, g:g + 1])
                            if g < 7:
                                emit_R(g + 1)
                            MM(scps[sc][:], Sels[qb][:, g, :], Rsb[rs][:], g == 0, g == 7, [t_Rsb[rs], t_Sels[qb]], [t_scps[sc]])
                        csl = slice(mp * 512, (mp + 1) * 512)
                        if mp < m:
                            ACTV(scores[:, csl], scps[sc][:], AF.Copy, [t_scps[sc]], [t_sc[mp]])
                        else:
                            RED(bs[:, 5:6], scps[sc][:], ALU.min, [t_scps[sc]], [t_bs])
                            TT(scores[:, csl], scps[sc][:], cmaskf[:], ALU.add, [t_scps[sc], t_cst], [t_sc[mp]])

                def thresh_steps(m, sp):
                    L = 512 * (m + 1)
                    scores, t_sc, maskb, t_mb = scores2[sp], t_sc2[sp], maskb2[sp], t_mb2[sp]
                    rd = [t_sc[i] for i in range(m + 1)]
                    steps = []

                    def s_init():
                        RED(bs[:, 1:2], scores[:, 0:L], ALU.max, rd, [t_bs])
                        if m > 0:
                            RED(bs[:, 0:1], scores[:, 0:L - 512], ALU.min, rd, [t_bs])
                            TT(bs[:, 0:1], bs[:, 0:1], bs[:, 5:6], ALU.min, [t_bs], [t_bs])
                        else:
                            TC(bs[:, 0:1], bs[:, 5:6], [t_bs], [t_bs])
                        TT(bs[:, 4:5], bs[:, 1:2], bs[:, 0:1], ALU.subtract, [t_bs], [t_bs])
                        TS(Hh[:], pow2[:], bs[:, 4:5], None, ALU.mult, None, [t_bs, t_cst], [t_H])
                    steps.append(s_init)

                    def mk_iter(k):
                        def s_iter():
                            TT(bs[:, 2:3], bs[:, 0:1], Hh[:, k:k + 1], ALU.add, [t_bs, t_H], [t_bs])
                            P.op("dve", lambda e: e.tensor_scalar(out=cntj[:, 0:L], in0=scores[:, 0:L], scalar1=bs[:, 2:3], scalar2=None,
                                                                  op0=ALU.is_ge, op1=ALU.add, accum_out=bs[:, 3:4], saturate=False),
                                 rd + [t_bs], [t_cntj, t_bs])
                            TS(bs[:, 4:5], bs[:, 3:4], float(TOPK) - 0.5, Hh[:, k:k + 1], ALU.is_ge, ALU.mult, [t_bs, t_H], [t_bs])
                            TT(bs[:, 0:1], bs[:, 0:1], bs[:, 4:5], ALU.add, [t_bs], [t_bs])
                        return s_iter
                    for k in range(NIT):
                        steps.append(mk_iter(k))

                    def s_fin():
                        TS(maskb[:, 0:L], scores[:, 0:L], bs[:, 0:1], NEGA, ALU.is_lt, ALU.mult, rd + [t_bs], [t_mb])
                        for c in range(m + 1):
                            STT(junk[:], maskb[:, c * 512:(c + 1) * 512], 200.0, posrow[:], ALU.mult, ALU.add, [t_mb, t_cst], [t_junk])
                            RED(bs[:, 6:7], junk[:], ALU.max, [t_junk], [t_bs])
                            if c == 0:
                                TC(bs[:, 7:8], bs[:, 6:7], [t_bs], [t_bs])
                            else:
                                STT(bs[:, 7:8], bs[:, 6:7], float(512 * c), bs[:, 7:8], ALU.add, ALU.max, [t_bs], [t_bs])
                        TC(sbf[:, 0:1], bs[:, 7:8], [t_bs], [t_sbf])
                        TS(U[:, 2:3], sbf[:, 0:1], -1.0, None, ALU.mult, None, [t_sbf], [t_U])
                        TT(U[:, 3:4], sbf[:, 0:1], bs[:, 7:8], ALU.subtract, [t_sbf, t_bs], [t_U])
                    steps.append(s_fin)
                    return steps

                def run_steps(steps, n=None):
                    k = 0
                    while steps and (n is None or k < n):
                        steps.pop(0)()
                        k += 1

                def thresh_pe(m, q, sp):
                    maskb, t_mb = maskb2[sp], t_mb2[sp]
                    sc = cn["sc"] % 2
                    cn["sc"] += 1
                    TR(scps[sc][0:4, 0:128], U[:, 0:4], identf[:], [t_U, tConst], [t_scps[sc]])
                    for h in range(8):
                        ACTV(QAt[64:68, h, q * 128:(q + 1) * 128], scps[sc][0:4, 0:128], AF.Copy, [t_scps[sc]], [t_QA], scale=2.0 ** -(h + 1))
                    for mp in range(m + 1):
                        for jp in range(4):
                            col = mp * 512 + jp * 128
                            TR(tpb[:, jp * 128:(jp + 1) * 128], maskb[:, col:col + 128], identb[:], [t_mb, t_cst], [t_tpb])
                        o_ = maskT[:, 4 * mp:4 * mp + 4, q * 128:(q + 1) * 128]
                        i_ = tpb[:, 0:512].rearrange("p (j t) -> p j t", j=4)
                        if mp % 2 == 0:
                            P.op("dve", lambda e, o_=o_, i_=i_: e.tensor_copy(out=o_, in_=i_, saturate=False), [t_tpb], [t_mT])
                        else:
                            P.op("act", lambda e, o_=o_, i_=i_: e.activation(out=o_, in_=i_, func=AF.Copy, saturate=False), [t_tpb], [t_mT])

                pend_fin = []

                def flush_fin(nmax=None):
                    nfl = 0
                    while pend_fin and (nmax is None or nfl < nmax):
                        nfl += 1
                        oi, idx16, m0 = pend_fin.pop(0)
                        RCP(rc[64:65, :], on[oi][64:65, :], [t_on[oi]], [t_rc])
                        MM(bcps[0:64, :], onesf[64:65, 0:64], rc[64:65, :], True, True, [t_rc, t_onesf], [t_bcps])
                        ot = idx16 % 2
                        TT(OTs[ot][:], on[oi][0:64, :], bcps[0:64, :], ALU.mult, [t_on[oi], t_bcps], [t_OTs[ot]])
                        DMA(OTd.ap()[idx16, :, m0 * 128:m0 * 128 + 512], OTs[ot][:], [t_OTs[ot]], [tOTd])

                def emit_pv(pt, vb, jp, mpl, c0, first, last, ob, idx16, m0):
                    MM(ops[ob][0:65, c0:512], Vb[vb][:, jp, mpl, :], PT[pt][:, c0:512], first, last, [t_PT[pt], t_V[vb]], [t_ops[ob]])
                    if last:
                        oi = cn["on"] % 2
                        cn["on"] += 1
                        ACTV(on[oi][:], ops[ob][0:65, :], AF.Copy, [t_ops[ob]], [t_on[oi]])
                        pend_fin.append((oi, idx16, m0))

                def attn(mixer, m0, heads, fin_now, inter=None):
                    isA = (mixer == 0)
                    Qt = QAt if isA else QFt
                    t_Q = t_QA if isA else t_QF
                    R = 68 if isA else 70
                    pend = []
                    for h in heads:
                        if fin_now:
                            flush_fin()
                        if inter:
                            run_steps(inter, 3)
                        ob = cn["op"] % 2
                        cn["op"] += 1
                        kb = vb = None
                        for mp in range(m0 + 4):
                            mpl = mp % 4
                            c0 = max(0, mp - m0) * 128
                            if mpl == 0:
                                nm = min(4, m0 + 4 - mp)
                                w = nm * 128
                                kb = cn["k"] % NKB
                                cn["k"] += 1
                                vb = cn["v"] % NVB
                                cn["v"] += 1
                                k0 = mp * 128
                                if isA:
                                    DMA(KAb[kb][0:66, :, 0:w], AGK3[h][:, 0:66, k0:k0 + w].rearrange("j r c -> r j c"),
                                        [tAGK[h]], [t_KA[kb]])
                                else:
                                    DMA(KFb[kb][0:64, :, 0:w],
                                        AGK3[8 + h // 2][:, (h % 2) * 64:(h % 2) * 64 + 64, k0:k0 + w].rearrange("j r c -> r j c"),
                                        [tAGK[8 + h // 2]], [t_KF[kb]])
                                    DMA(KFb[kb][64:67, :, 0:w], FKd.ap()[h, :, :, k0:k0 + w], [tFKd], [t_KF[kb]])
                                hh = h if isA else 8 + h
                                for jj in range(4):
                                    DMA(Vb[vb][:, jj, 0:nm, 0:64], AGV5[hh // 2][jj, :, hh % 2, mp:mp + nm, :],
                                        [tAGV[hh // 2]], [t_V[vb]])
                            Kt = KAb[kb] if isA else KFb[kb]
                            t_K = t_KA[kb] if isA else t_KF[kb]
                            for jp in range(4):
                                sb_ = cn["sp"] % 4
                                cn["sp"] += 1
                                diag = (not isA) and (mp >= m0)
                                MM(sps[sb_][:, c0:512], Kt[0:R, jp, mpl * 128:(mpl + 1) * 128], Qt[0:R, h, c0:512], True, not (isA or diag),
                                   [t_K, t_Q], [t_sps[sb_]])
                                if isA:
                                    MM(sps[sb_][:, c0:512], identS[:], maskT[:, 4 * mp + jp, c0:512], False, True, [t_mT, t_idS], [t_sps[sb_]])
                                elif diag:
                                    MM(sps[sb_][:, c0:c0 + 128], identb[:], cmT[:, jp, :], False, True, [t_cst], [t_sps[sb_]])
                                pt = cn["pt"] % NPT
                                cn["pt"] += 1
                                ACTV(PT[pt][:, c0:512], sps[sb_][:, c0:512], AF.Exp, [t_sps[sb_]], [t_PT[pt]])
                                first = (mp == 0 and jp == 0)
                                last = (mp == m0 + 3 and jp == 3)
                                pend.append((pt, vb, jp, mpl, c0, first, last, ob, mixer * 8 + h, m0))
                                if len(pend) > LAG:
                                    emit_pv(*pend.pop(0))
                    while pend:
                        emit_pv(*pend.pop(0))

                seq = [4 * B + q for B in range(4) for q in (3, 2, 1, 0)]
                load_qi(seq[0])
                idx(seq[0], 0)
                cur = thresh_steps(seq[0], 0)
                for k, m in enumerate(seq):
                    B, q = divmod(m, 4)
                    m0 = 4 * B
                    sp = k % 2
                    if q == 3:
                        load_qbatch(m0)
                    run_steps(cur, 5)
                    flush_fin(1)
                    run_steps(cur, 5)
                    flush_fin()
                    run_steps(cur)
                    if k + 1 < 16:
                        load_qi(seq[k + 1])
                        idx(seq[k + 1], (k + 1) % 2)
                    attn(1, m0, [2 * (3 - q), 2 * (3 - q) + 1], False)
                    thresh_pe(m, q, sp)
                    cur = thresh_steps(seq[k + 1], (k + 1) % 2) if k + 1 < 16 else []
                    if q == 0:
                        flush_fin()
                        attn(0, m0, list(range(8)), True, inter=cur)
                        flush_fin()
                P.barrier()
                P.flush(block)

        def phase5():
            with ExitStack() as ph:
                Ep = ph.enter_context
                nb = norm_bufs(Ep, "p5")
                h2 = Ep(nc.sbuf_tensor("p5h2", [128, 8, 512], BF16))
                OT = Ep(nc.sbuf_tensor("p5OT", [128, 8, 512], BF16))
                mg = Ep(nc.sbuf_tensor("p5mg", [128, 8, 512], BF16))
                wga = Ep(nc.sbuf_tensor("p5wga", [128, 8, D], BF16))
                wgb_ = Ep(nc.sbuf_tensor("p5wgb", [128, 8, D], BF16))
                wbA = Ep(nc.sbuf_tensor("p5wbA", [128, 4, D], BF16))
                wbF = Ep(nc.sbuf_tensor("p5wbF", [128, 4, D], BF16))
                wo = Ep(nc.sbuf_tensor("p5wo", [128, 8, D], BF16))
                identb = Ep(nc.sbuf_tensor("p5idb", [128, 128], BF16))
                Ot = [Ep(nc.sbuf_tensor("p5Ot%d" % i, [128, D], BF16)) for i in range(2)]
                sA = Ep(nc.sbuf_tensor("p5sA", [128, 512], F32))
                sB = Ep(nc.sbuf_tensor("p5sB", [128, 512], F32))
                t1 = Ep(nc.sbuf_tensor("p5t1", [128, 512], F32))
                t2 = Ep(nc.sbuf_tensor("p5t2", [128, 512], F32))
                psA = Ep(nc.psum_tensor("p5psA", [128, 512], F32))
                psB = Ep(nc.psum_tensor("p5psB", [128, 512], F32))
                psyA = Ep(nc.psum_tensor("p5psyA", [128, 512], F32))
                psyB = Ep(nc.psum_tensor("p5psyB", [128, 512], F32))
                pso = Ep(nc.psum_tensor("p5pso", [128, 512], F32))
                ptr = Ep(nc.psum_tensor("p5ptr", [128, 1024], BF16))
                block = Ep(nc.Block())
                t_w, t_idb, t_sA, t_sB, t_t1, t_t2, t_psA, t_psB, t_psyA, t_psyB, t_pso, t_ptr = Ts(12)
                t_h2, t_OT, t_mg, t_Ot = Ts(8), Ts(8), Ts(8), Ts(2)
                DMA(xT[:, :, :].rearrange("p a t -> p (a t)"), XTd.ap(), [tXTd], [xT_T[fc][tt] for fc in range(8) for tt in range(4)])
                DMA(identb[:], identb_d, [], [t_idb])
                DMA(wga[:], w_in[:, 3664:4688].rearrange("(kc p) f -> p kc f", p=128), [], [t_w], q="pool")
                DMA(wgb_[:], w_in[:, 4688:5712].rearrange("(kc p) f -> p kc f", p=128), [], [t_w], q="pool")
                DMA(wbA[:], w_brA.rearrange("(kc p) f -> p kc f", p=128), [], [t_w], q="pool")
                DMA(wbF[:], w_brF.rearrange("(kc p) f -> p kc f", p=128), [], [t_w], q="pool")
                DMA(wo[:], w_out.rearrange("(kc p) f -> p kc f", p=128), [], [t_w], q="pool")
                for tt in range(4):
                    emit_norm(1, tt, nb, lambda kc: h2[:, kc, :], lambda kc: t_h2[kc])
                    for c in range(8):
                        DMA(OT[:, c, :], OTd.ap()[2 * c:2 * c + 2, :, tok(tt)].rearrange("h d t -> (h d) t"), [tOTd], [t_OT[c]])
                    for fc in range(8):
                        fs = slice(fc * 128, (fc + 1) * 128)
                        for kc in range(8):
                            MM(psA[:], wga[:, kc, fs], h2[:, kc, :], kc == 0, kc == 7, [t_w, t_h2[kc]], [t_psA])
                        for kc in range(8):
                            MM(psB[:], wgb_[:, kc, fs], h2[:, kc, :], kc == 0, kc == 7, [t_w, t_h2[kc]], [t_psB])
                        for c in range(4):
                            MM(psyA[:], wbA[:, c, fs], OT[:, c, :], c == 0, c == 3, [t_w, t_OT[c]], [t_psyA])
                        for c in range(4):
                            MM(psyB[:], wbF[:, c, fs], OT[:, 4 + c, :], c == 0, c == 3, [t_w, t_OT[4 + c]], [t_psyB])
                        ACTV(sA[:], psA[:], AF.Sigmoid, [t_psA], [t_sA])
                        ACTV(sB[:], psB[:], AF.Sigmoid, [t_psB], [t_sB])
                        TT(t1[:], sA[:], psyA[:], ALU.mult, [t_sA, t_psyA], [t_t1])
                        TT(t2[:], sB[:], psyB[:], ALU.mult, [t_sB, t_psyB], [t_t2])
                        TT(mg[:, fc, :], t1[:], t2[:], ALU.add, [t_t1, t_t2], [t_mg[fc]])
                    for f2 in range(8):
                        fs = slice(f2 * 128, (f2 + 1) * 128)
                        for fc in range(8):
                            MM(pso[:], wo[:, fc, fs], mg[:, fc, :], fc == 0, fc == 7, [t_w, t_mg[fc]], [t_pso])
                        STT(xT[:, f2, tok(tt)], pso[:], AG[:, 3, f2:f2 + 1], xT[:, f2, tok(tt)], ALU.mult, ALU.add,
                            [t_pso, tAG], [xT_T[f2][tt]])
                P.barrier()
                P.flush(block)

        if STAGES >= 3:
            phase3()
            if not (DBG & 1):
                phase35()
        if STAGES >= 4:
            xstack.close()
            phase4()
            xstack = ExitStack()
            xT = xstack.enter_context(nc.sbuf_tensor("xT2", [128, 8, NT], F32))
            phase5()

        if STAGES >= 9:
            ffn("f2", 2)

        with ExitStack() as ph:
            Ep = ph.enter_context
            ob = [Ep(nc.sbuf_tensor("ob%d" % i, [128, D], F32)) for i in range(2)]
            tps = [Ep(nc.psum_tensor("otps%d" % i, [128, 512], F32)) for i in range(4)]
            block = Ep(nc.Block())
            t_ob, t_tps = Ts(2), Ts(4)
            t_out = T()
            ev = 0
            for m in range(16):
                b = m % 2
                for half in range(2):
                    pb = (2 * m + half) % 4
                    for q in range(4):
                        fc = half * 4 + q
                        P.op("pe", lambda e, pb=pb, q=q, fc=fc, m=m: e.transpose(
                            tps[pb][:, q * 128:(q + 1) * 128], xT[:, fc, m * 128:(m + 1) * 128], identf[:]),
                            reads=[xT_T[fc][m // 4], tConst], writes=[t_tps[pb]])
                    if ev % 2 == 0:
                        P.op("dve", lambda e, b=b, pb=pb, half=half: e.tensor_copy(
                            out=ob[b][:, half * 512:(half + 1) * 512], in_=tps[pb][:]), reads=[t_tps[pb]], writes=[t_ob[b]])
                    else:
                        P.op("act", lambda e, b=b, pb=pb, half=half: e.activation(
                            out=ob[b][:, half * 512:(half + 1) * 512], in_=tps[pb][:], func=AF.Copy),
                            reads=[t_tps[pb]], writes=[t_ob[b]])
                    ev += 1
                P.dma("sp", lambda e, b=b, m=m: e.dma_start(out=out_d[m * 128:(m + 1) * 128, :], in_=ob[b][:]),
                      reads=[t_ob[b]], writes=[t_out])
            P.barrier()
            P.flush(block)
        xstack.close()
    return nc


def _bf(a):
    return np.ascontiguousarray(a.astype(ml_dtypes.bfloat16))


def make_in_maps(inp):
    f = lambda a: np.ascontiguousarray(np.asarray(a, dtype=np.float32))
    x = f(inp["x"])
    c = f(inp["c"])
    colT = lambda v, n: np.ascontiguousarray(v.reshape(n, 128).T)
    shared = {
        "ada_w": f(inp["ada_w"])[0],
        "ada_bT": colT(f(inp["ada_b"])[0], 72),
        "nT": np.ascontiguousarray(np.stack([colT(f(inp[k])[0], 8) for k in ("norm1_g", "norm2_g", "norm3_g")], axis=1)),
        "f1wg": f(inp["ffn1_wg"])[0], "f1wu": f(inp["ffn1_wu"])[0], "f1wd": f(inp["ffn1_wd"])[0],
        "f2wg": f(inp["ffn2_wg"])[0], "f2wu": f(inp["ffn2_wu"])[0], "f2wd": f(inp["ffn2_wd"])[0],
        "identf": np.eye(128, dtype=np.float32),
        "onesb": _bf(np.ones((128, 128), np.float32)),
        "w_in": f(inp["w_in"])[0],
        "bfT": np.ascontiguousarray(f(inp["b_forget"])[0].reshape(8, 1)),
        "gnT": np.ascontiguousarray(np.tile(np.stack([f(inp[k])[0] for k in ("qn_dsa", "kn_dsa", "qn_fox", "kn_fox")], axis=1), (2, 1))),
        "onesbd": _bf(np.kron(np.eye(2, dtype=np.float32), np.ones((64, 64), np.float32))),
        "w_brA": f(inp["w_br_dsa"])[0], "w_brF": f(inp["w_br_fox"])[0], "w_out": f(inp["w_out"])[0],
        "identb": _bf(np.eye(128, dtype=np.float32)),
    }
    sel = np.zeros((128, 8, 128), np.float32)
    for i in range(8):
        for t16 in range(16):
            for g in range(8):
                sel[i * 16 + t16, g, 16 * g + t16] = 1.0
    shared["sel"] = _bf(sel)
    shared["pow2"] = np.ascontiguousarray(np.tile((0.5 ** np.arange(1, NIT + 1, dtype=np.float64)).astype(np.float32)[None, :], (128, 1)))
    shared["posrow"] = np.ascontiguousarray(np.tile(np.arange(512, dtype=np.float32)[None, :], (128, 1)))
    maps = []
    tq = np.arange(128)[:, None]
    sk = np.arange(128)[None, :]
    for core in range(8):
        b, j = core // 4, core % 4
        xo = x[b].reshape(16, 4, 128, D)[:, j].reshape(NT, D)
        m = dict(shared)
        m["x_own"] = np.ascontiguousarray(xo)
        m["cT"] = colT(c[b], 8)
        vis = np.zeros((128, 4, 128), bool)
        for jp in range(4):
            vis[:, jp, :] = True if jp < j else ((sk <= tq) if jp == j else False)
        m["cmaskf"] = np.ascontiguousarray(np.where(vis, 0.0, -1e30).astype(np.float32).reshape(128, 512))
        m["cmT"] = _bf(np.ascontiguousarray(np.where(vis, 0.0, NEG).astype(np.float32).transpose(2, 1, 0)))
        pos = ((4 * np.arange(16)[:, None] + j) * 128 + np.arange(128)[None, :]).reshape(-1)
        m["posK"] = _bf(np.stack([(pos // 64) * 64, pos % 64]).astype(np.float32))
        sj = np.zeros((8, 4), np.float32)
        sj[:, j] = 1.0
        m["seljF"] = sj
        maps.append(m)
    return maps


_NC = None


def kernel(**inputs):
    global _NC
    if _NC is None:
        _NC = build()
    maps = make_in_maps(inputs)
    res = run_bass_kernel_spmd(_NC, maps, core_ids=list(range(8)))
    out = np.zeros((2, S, D), np.float32)
    for core in range(8):
        b, j = core // 4, core % 4
        o = np.asarray(res.results[core]["out"], dtype=np.float32).reshape(16, 128, D)
        out[b].reshape(16, 4, 128, D)[:, j] = o
    return out
```

```python
from contextlib import ExitStack
import numpy as np
import ml_dtypes
import concourse.bass as bass
import concourse.mybir as mybir
from concourse.bass_utils import run_bass_kernel_spmd

F32 = mybir.dt.float32
BF16 = mybir.dt.bfloat16
AF = mybir.ActivationFunctionType
ALU = mybir.AluOpType
AX = mybir.AxisListType

D = 1024
S = 8192
NT = 2048
DFF = 2816
NJ = 22
INW = 5712
EPS = 1e-6
NIT = 16
TOPK = 256
NEG = -30000.0
NEGA = -240.0

STAGES = 9
import os as _os
DBG = int(_os.environ.get("KDBG", "0"))


class T:
    __slots__ = ("w", "r")

    def __init__(self):
        self.w = None
        self.r = {}


def Ts(n):
    return [T() for _ in range(n)]


class Prog:
    ENG = ["pe", "act", "dve", "pool", "sp"]

    def __init__(self, nc, sems):
        self.nc = nc
        self.sem = sems
        self.cnt = {k: 0 for k in sems}
        self.seen = {n: {} for n in self.ENG}
        self.q = {n: [] for n in self.ENG}
        self.dn = {}

    def _wait(self, eng, key, val):
        if val <= 0 or self.seen[eng].get(key, 0) >= val:
            return
        self.seen[eng][key] = val
        sem = self.sem[key]
        self.q[eng].append(lambda e, sem=sem, val=val: e.wait_ge(sem, val))

    def _deps(self, eng, reads, writes, selfkey):
        for b in reads:
            if b.w is not None and not (b.w[0] == "pe" and selfkey == "pe"):
                self._wait(eng, *b.w)
        for b in writes:
            if b.w is not None and not (b.w[0] == "pe" and selfkey == "pe"):
                self._wait(eng, *b.w)
            for k, v in b.r.items():
                if not (k == "pe" and selfkey == "pe"):
                    self._wait(eng, k, v)

    def op(self, eng, fn, reads=(), writes=()):
        self._deps(eng, reads, writes, eng)
        self.cnt[eng] += 1
        c = self.cnt[eng]
        sem = self.sem[eng]
        self.q[eng].append(lambda e, fn=fn, sem=sem: fn(e).then_inc(sem, 1))
        for b in reads:
            b.r[eng] = c
        for b in writes:
            b.w = (eng, c)
            b.r = {}

    NDS = 16

    def dma(self, eng, fn, reads=(), writes=()):
        n = self.dn.get(eng, 0)
        self.dn[eng] = n + 1
        key = "d%s%d" % (eng, n % self.NDS)
        self._wait(eng, key, self.cnt[key])
        self._deps(eng, reads, writes, key)
        self.cnt[key] += 16
        c = self.cnt[key]
        sem = self.sem[key]
        self.q[eng].append(lambda e, fn=fn, sem=sem: fn(e).then_inc(sem, 16))
        for b in reads:
            b.r[key] = c
        for b in writes:
            b.w = (key, c)
            b.r = {}

    def coll(self, fn, reads=(), writes=()):
        self._deps("pool", reads, writes, "cc")
        self.cnt["cc"] += 1
        c = self.cnt["cc"]
        sem = self.sem["cc"]
        self.q["pool"].append(lambda e, fn=fn, sem=sem: fn(e).then_inc(sem, 1))
        for b in reads:
            b.r["cc"] = c
        for b in writes:
            b.w = ("cc", c)
            b.r = {}

    def barrier(self):
        for eng in self.ENG:
            for k, v in self.cnt.items():
                self._wait(eng, k, v)

    def flush(self, block):
        q = self.q
        self.q = {n: [] for n in self.ENG}

        @block.tensor
        def _(e):
            for f in q["pe"]:
                f(e)

        @block.scalar
        def _(e):
            for f in q["act"]:
                f(e)

        @block.vector
        def _(e):
            for f in q["dve"]:
                f(e)

        @block.gpsimd
        def _(e):
            for f in q["pool"]:
                f(e)

        @block.sync
        def _(e):
            for f in q["sp"]:
                f(e)


def build():
    nc = bass.Bass("TRN2", target_bir_lowering=False)

    def din(name, shape, dt=F32):
        return nc.dram_tensor(name, shape, dt, kind="ExternalInput").ap()

    x_own = din("x_own", [NT, D])
    cT_d = din("cT", [128, 8])
    ada_w = din("ada_w", [D, 9 * D])
    ada_bT = din("ada_bT", [128, 72])
    nT_d = din("nT", [128, 3, 8])
    ffw = {}
    for t in ("f1", "f2"):
        ffw[t] = (din(t + "wg", [D, DFF]), din(t + "wu", [D, DFF]), din(t + "wd", [DFF, D]))
    identf_d = din("identf", [128, 128])
    onesb_d = din("onesb", [128, 128], BF16)
    out_d = nc.dram_tensor("out", [NT, D], F32, kind="ExternalOutput").ap()
    w_in = din("w_in", [D, INW])
    bfT_d = din("bfT", [8, 1])
    gn_d = din("gnT", [128, 4])
    onesbd_d = din("onesbd", [128, 128], BF16)
    w_brA = din("w_brA", [512, D])
    w_brF = din("w_brF", [512, D])
    w_out = din("w_out", [D, D])
    identb_d = din("identb", [128, 128], BF16)
    sel_d = din("sel", [128, 8, 128], BF16)
    cmaskf_d = din("cmaskf", [128, 512])
    cmT_d = din("cmT", [128, 4, 128], BF16)
    posK_d = din("posK", [2, NT], BF16)
    seljF_d = din("seljF", [8, 4])
    pow2_d = din("pow2", [128, NIT])
    posrow_d = din("posrow", [128, 512])
    NKC = 13
    NVC = 8
    XKc = [nc.dram_tensor("XK%d" % i, [128, NT], BF16) for i in range(NKC)]
    AGKc = [nc.dram_tensor("AGK%d" % i, [4 * 128, NT], BF16) for i in range(NKC)]
    XVc = [nc.dram_tensor("XV%d" % i, [128, 2048], BF16) for i in range(NVC)]
    AGVc = [nc.dram_tensor("AGV%d" % i, [4 * 128, 2048], BF16) for i in range(NVC)]
    XL = nc.dram_tensor("XL", [8, NT], F32)
    AGL = nc.dram_tensor("AGL", [4 * 8, NT], F32)
    QAd = nc.dram_tensor("QAd", [8, 64, NT], BF16)
    QFd = nc.dram_tensor("QFd", [8, 64, NT], BF16)
    QId = nc.dram_tensor("QId", [64, 8, NT], BF16)
    Wd = nc.dram_tensor("Wd", [8, NT], F32)
    FKd = nc.dram_tensor("FKd", [8, 3, 4, NT], BF16)
    FQd = nc.dram_tensor("FQd", [8, 3, NT], BF16)
    OTd = nc.dram_tensor("OTd", [16, 64, NT], BF16)
    XTd = nc.dram_tensor("XTd", [128, 8 * NT], F32)
    RG = [[0, 1, 2, 3], [4, 5, 6, 7]]

    with ExitStack() as top:
        E = top.enter_context
        keys = ["pe", "act", "dve", "pool", "sp", "cc"]
        keys += ["dsp%d" % i for i in range(Prog.NDS)] + ["dpool%d" % i for i in range(Prog.NDS)]
        sems = {k: E(nc.semaphore("s_" + k)) for k in keys}
        P = Prog(nc, sems)
        xT_T = [Ts(4) for _ in range(8)]
        modT = E(nc.sbuf_tensor("modT", [128, 72], F32))
        AG = E(nc.sbuf_tensor("AG", [128, 6, 8], F32))
        identf = E(nc.sbuf_tensor("identf_s", [128, 128], F32))
        onesb = E(nc.sbuf_tensor("onesb_s", [128, 128], BF16))
        tMod, tAG, tConst = T(), T(), T()
        xstack = ExitStack()
        xT = xstack.enter_context(nc.sbuf_tensor("xT", [128, 8, NT], F32))

        def tok(tt):
            return slice(tt * 512, (tt + 1) * 512)

        with ExitStack() as ph:
            Ep = ph.enter_context
            cT = Ep(nc.sbuf_tensor("cT_s", [128, 8], F32))
            csil = Ep(nc.sbuf_tensor("csil", [128, 8], BF16))
            abT = Ep(nc.sbuf_tensor("abT", [128, 72], F32))
            nT = Ep(nc.sbuf_tensor("nT_s", [128, 3, 8], F32))
            adaw = [Ep(nc.sbuf_tensor("adaw%d" % i, [128, 8, 1024], BF16)) for i in range(2)]
            xs = [Ep(nc.sbuf_tensor("xs%d" % i, [128, D], F32)) for i in range(2)]
            modps = Ep(nc.psum_tensor("modps", [128, 512], F32))
            tps = [Ep(nc.psum_tensor("tps%d" % i, [128, 512], F32)) for i in range(4)]
            block = Ep(nc.Block())
            t_c, t_cs, t_ab, t_n, t_mps = Ts(5)
            t_adaw, t_xs, t_tps = Ts(2), Ts(2), Ts(4)
            P.dma("sp", lambda e: e.dma_start(out=identf[:], in_=identf_d), writes=[tConst])
            P.dma("sp", lambda e: e.dma_start(out=onesb[:], in_=onesb_d), writes=[tConst])
            P.dma("sp", lambda e: e.dma_start(out=cT[:], in_=cT_d), writes=[t_c])
            P.dma("sp", lambda e: e.dma_start(out=abT[:], in_=ada_bT), writes=[t_ab])
            P.dma("sp", lambda e: e.dma_start(out=nT[:], in_=nT_d), writes=[t_n])
            P.op("act", lambda e: e.activation(out=csil[:], in_=cT[:], func=AF.Silu), reads=[t_c], writes=[t_cs])
            for gi in range(9):
                b = gi % 2
                P.dma("pool", lambda e, gi=gi, b=b: e.dma_start(
                    out=adaw[b][:], in_=ada_w[:, gi * 1024:(gi + 1) * 1024].rearrange("(kc p) f -> p kc f", p=128)),
                    writes=[t_adaw[b]])
                for fc in range(8):
                    col = gi * 8 + fc
                    for kc in range(8):
                        P.op("pe", lambda e, b=b, fc=fc, kc=kc, col=col: e.matmul(
                            modps[:, col:col + 1], adaw[b][:, kc, fc * 128:(fc + 1) * 128], csil[:, kc:kc + 1],
                            start=(kc == 0), stop=(kc == 7)), reads=[t_adaw[b], t_cs], writes=[t_mps])
            P.op("dve", lambda e: e.tensor_tensor(out=modT[:], in0=modps[:, 0:72], in1=abT[:], op=ALU.add),
                 reads=[t_mps, t_ab], writes=[tMod])
            for k in range(3):
                P.op("dve", lambda e, k=k: e.scalar_tensor_tensor(
                    out=AG[:, 2 * k, :], in0=modT[:, (3 * k + 1) * 8:(3 * k + 2) * 8], scalar=1.0, in1=nT[:, k, :],
                    op0=ALU.add, op1=ALU.mult), reads=[tMod, t_n], writes=[tAG])
                gsc = 1.0 if k == 1 else 0.5
                P.op("dve", lambda e, k=k, gsc=gsc: e.tensor_scalar(
                    out=AG[:, 2 * k + 1, :], in0=modT[:, (3 * k + 2) * 8:(3 * k + 3) * 8], scalar1=gsc, scalar2=None,
                    op0=ALU.mult), reads=[tMod], writes=[tAG])
            ev = 0
            for m in range(16):
                b = m % 2
                P.dma("sp", lambda e, m=m, b=b: e.dma_start(out=xs[b][:], in_=x_own[m * 128:(m + 1) * 128, :]),
                      writes=[t_xs[b]])
                for half in range(2):
                    pb = (2 * m + half) % 4
                    for q in range(4):
                        fc = half * 4 + q
                        P.op("pe", lambda e, b=b, pb=pb, q=q, fc=fc: e.transpose(
                            tps[pb][:, q * 128:(q + 1) * 128], xs[b][:, fc * 128:(fc + 1) * 128], identf[:]),
                            reads=[t_xs[b], tConst], writes=[t_tps[pb]])
                    wr = [xT_T[fc][m // 4] for fc in range(half * 4, half * 4 + 4)]
                    src = lambda pb=pb: tps[pb][:, :].rearrange("p (q t) -> p q t", q=4)
                    dst = lambda half=half, m=m: xT[:, half * 4:(half + 1) * 4, m * 128:(m + 1) * 128]
                    if ev % 2 == 0:
                        P.op("dve", lambda e, src=src, dst=dst: e.tensor_copy(out=dst(), in_=src()),
                             reads=[t_tps[pb]], writes=wr)
                    else:
                        P.op("act", lambda e, src=src, dst=dst: e.activation(out=dst(), in_=src(), func=AF.Copy),
                             reads=[t_tps[pb]], writes=wr)
                    ev += 1
            P.barrier()
            P.flush(block)

        def ffn(tag, k):
            wg, wu, wd = ffw[tag]
            with ExitStack() as ph:
                Ep = ph.enter_context
                hT = Ep(nc.sbuf_tensor(tag + "hT", [128, 8, NT], BF16))
                actT = Ep(nc.sbuf_tensor(tag + "actT", [128, 8, NT], BF16))
                sq = Ep(nc.sbuf_tensor(tag + "sq", [128, 8, 512], BF16))
                rt = Ep(nc.sbuf_tensor(tag + "rt", [128, 512], F32))
                rstd = Ep(nc.sbuf_tensor(tag + "rstd", [128, 512], F32))
                tmp = [Ep(nc.sbuf_tensor(tag + "tmp%d" % i, [128, 512], F32)) for i in range(2)]
                sgb = [Ep(nc.sbuf_tensor(tag + "sg%d" % i, [128, 512], F32)) for i in range(2)]
                wgb = [Ep(nc.sbuf_tensor(tag + "wg%d" % i, [128, 8, 128], BF16)) for i in range(2)]
                wub = [Ep(nc.sbuf_tensor(tag + "wu%d" % i, [128, 8, 128], BF16)) for i in range(2)]
                wdb = [Ep(nc.sbuf_tensor(tag + "wd%d" % i, [128, 8, 128], BF16)) for i in range(2)]
                pst = Ep(nc.psum_tensor(tag + "pst", [128, 512], F32))
                psg = [Ep(nc.psum_tensor(tag + "psg%d" % i, [128, 512], F32)) for i in range(2)]
                psu = [Ep(nc.psum_tensor(tag + "psu%d" % i, [128, 512], F32)) for i in range(2)]
                psd = [Ep(nc.psum_tensor(tag + "psd%d" % i, [128, 512], F32)) for i in range(2)]
                block = Ep(nc.Block())
                t_hT = [Ts(4) for _ in range(8)]
                t_act = [Ts(4) for _ in range(8)]
                t_sq, t_rt, t_rstd, t_pst = Ts(4)
                t_tmp, t_sg, t_wg, t_wu, t_wd, t_psg, t_psu, t_psd = (Ts(2) for _ in range(8))
                Acol = lambda kc: AG[:, 2 * k, kc:kc + 1]
                Gcol = lambda kc: AG[:, 2 * k + 1, kc:kc + 1]
                shcol = lambda kc: modT[:, 3 * k * 8 + kc:3 * k * 8 + kc + 1]
                for tt in range(4):
                    P.op("act", lambda e, tt=tt: e.activation(out=sq[:], in_=xT[:, :, tok(tt)], func=AF.Square),
                         reads=[xT_T[fc][tt] for fc in range(8)], writes=[t_sq])
                    for kc in range(8):
                        P.op("pe", lambda e, kc=kc: e.matmul(pst[:], onesb[:], sq[:, kc, :], start=(kc == 0), stop=(kc == 7)),
                             reads=[t_sq, tConst], writes=[t_pst])
                    P.op("act", lambda e: e.activation(out=rt[:], in_=pst[:], func=AF.Sqrt, scale=1.0 / D, bias=EPS),
                         reads=[t_pst], writes=[t_rt])
                    P.op("dve", lambda e: e.reciprocal(out=rstd[:], in_=rt[:]), reads=[t_rt], writes=[t_rstd])
                    for kc in range(8):
                        b = kc % 2
                        P.op("dve", lambda e, kc=kc, b=b, tt=tt: e.scalar_tensor_tensor(
                            out=tmp[b][:], in0=xT[:, kc, tok(tt)], scalar=Acol(kc), in1=rstd[:], op0=ALU.mult, op1=ALU.mult),
                            reads=[xT_T[kc][tt], t_rstd, tAG], writes=[t_tmp[b]])
                        P.op("act", lambda e, kc=kc, b=b, tt=tt: e.activation(
                            out=hT[:, kc, tok(tt)], in_=tmp[b][:], func=AF.Identity, bias=shcol(kc)),
                            reads=[t_tmp[b], tMod], writes=[t_hT[kc][tt]])
                groups = [list(range(0, 8)), list(range(8, 15)), list(range(15, 22))]
                wi = 0
                di = 0
                pi = 0
                for js in groups:
                    for jj, j in enumerate(js):
                        b = wi % 2
                        wi += 1
                        P.dma("pool", lambda e, b=b, j=j: e.dma_start(
                            out=wgb[b][:], in_=wg[:, j * 128:(j + 1) * 128].rearrange("(kc p) f -> p kc f", p=128)),
                            writes=[t_wg[b]])
                        P.dma("pool", lambda e, b=b, j=j: e.dma_start(
                            out=wub[b][:], in_=wu[:, j * 128:(j + 1) * 128].rearrange("(kc p) f -> p kc f", p=128)),
                            writes=[t_wu[b]])
                        for tt in range(4):
                            pb = pi % 2
                            pi += 1
                            for kc in range(8):
                                P.op("pe", lambda e, b=b, pb=pb, kc=kc, tt=tt: e.matmul(
                                    psg[pb][:], wgb[b][:, kc, :], hT[:, kc, tok(tt)], start=(kc == 0), stop=(kc == 7)),
                                    reads=[t_wg[b], t_hT[kc][tt]], writes=[t_psg[pb]])
                            for kc in range(8):
                                P.op("pe", lambda e, b=b, pb=pb, kc=kc, tt=tt: e.matmul(
                                    psu[pb][:], wub[b][:, kc, :], hT[:, kc, tok(tt)], start=(kc == 0), stop=(kc == 7)),
                                    reads=[t_wu[b], t_hT[kc][tt]], writes=[t_psu[pb]])
                            P.op("act", lambda e, pb=pb: e.activation(out=sgb[pb][:], in_=psg[pb][:], func=AF.Silu),
                                 reads=[t_psg[pb]], writes=[t_sg[pb]])
                            P.op("dve", lambda e, pb=pb, jj=jj, tt=tt: e.tensor_tensor(
                                out=actT[:, jj, tok(tt)], in0=sgb[pb][:], in1=psu[pb][:], op=ALU.mult),
                                reads=[t_sg[pb], t_psu[pb]], writes=[t_act[jj][tt]])
                    n = len(js)
                    j0 = js[0]
                    for d in range(8):
                        b = di % 2
                        di += 1
                        P.dma("pool", lambda e, b=b, d=d, n=n, j0=j0: e.dma_start(
                            out=wdb[b][:, 0:n, :],
                            in_=wd[j0 * 128:(j0 + n) * 128, d * 128:(d + 1) * 128].rearrange("(jj p) f -> p jj f", p=128)),
                            writes=[t_wd[b]])
                        for tt in range(4):
                            pb = (d * 4 + tt) % 2
                            for jj in range(n):
                                P.op("pe", lambda e, b=b, pb=pb, jj=jj, tt=tt, n=n: e.matmul(
                                    psd[pb][:], wdb[b][:, jj, :], actT[:, jj, tok(tt)], start=(jj == 0), stop=(jj == n - 1)),
                                    reads=[t_wd[b], t_act[jj][tt]], writes=[t_psd[pb]])
                            P.op("dve", lambda e, pb=pb, d=d, tt=tt: e.scalar_tensor_tensor(
                                out=xT[:, d, tok(tt)], in0=psd[pb][:], scalar=Gcol(d), in1=xT[:, d, tok(tt)],
                                op0=ALU.mult, op1=ALU.add),
                                reads=[t_psd[pb], tAG], writes=[xT_T[d][tt]])
                P.barrier()
                P.flush(block)

        if STAGES >= 1:
            ffn("f1", 0)

        def MM(o, l, r, st, sp_, rd, wr):
            P.op("pe", lambda e: e.matmul(o, l, r, start=st, stop=sp_), rd, wr)

        def TR(o, i, idn, rd, wr):
            P.op("pe", lambda e: e.transpose(o, i, idn), rd, wr)

        def ACTV(o, i, f, rd, wr, scale=None, bias=None):
            kw = {}
            if scale is not None:
                kw["scale"] = scale
            if bias is not None:
                kw["bias"] = bias
            P.op("act", lambda e: e.activation(out=o, in_=i, func=f, **kw), rd, wr)

        def TS(o, i, s1, s2, op0, op1, rd, wr, acc=None):
            kw = {}
            if op1 is not None:
                kw["op1"] = op1
            if acc is not None:
                kw["accum_out"] = acc
            P.op("dve", lambda e: e.tensor_scalar(out=o, in0=i, scalar1=s1, scalar2=s2, op0=op0, **kw), rd, wr)

        def TT(o, a, b, op, rd, wr):
            P.op("dve", lambda e: e.tensor_tensor(out=o, in0=a, in1=b, op=op), rd, wr)

        def STT(o, a, s, b, op0, op1, rd, wr):
            P.op("dve", lambda e: e.scalar_tensor_tensor(out=o, in0=a, scalar=s, in1=b, op0=op0, op1=op1), rd, wr)

        def TC(o, i, rd, wr):
            P.op("dve", lambda e: e.tensor_copy(out=o, in_=i), rd, wr)

        def RED(o, i, op, rd, wr):
            P.op("dve", lambda e: e.tensor_reduce(out=o, in_=i, axis=AX.X, op=op), rd, wr)

        def RCP(o, i, rd, wr):
            P.op("dve", lambda e: e.reciprocal(out=o, in_=i), rd, wr)

        def MSET(o, v, wr):
            P.op("dve", lambda e: e.memset(o, v), (), wr)

        def DMA(o, i, rd, wr, q="sp", **kw):
            P.dma(q, lambda e: e.dma_start(out=o, in_=i, **kw), rd, wr)

        evc = [0]

        def COPY(o, i, rd, wr):
            if evc[0] % 2 == 0:
                TC(o, i, rd, wr)
            else:
                ACTV(o, i, AF.Copy, rd, wr)
            evc[0] += 1

        def emit_norm(k, tt, nb, dst, t_dst):
            sq, rt, rstd, tmp, pst, t_sq, t_rt, t_rstd, t_tmp, t_pst = nb
            ACTV(sq[:], xT[:, :, tok(tt)], AF.Square, [xT_T[fc][tt] for fc in range(8)], [t_sq])
            for kc in range(8):
                MM(pst[:], onesb[:], sq[:, kc, :], kc == 0, kc == 7, [t_sq, tConst], [t_pst])
            ACTV(rt[:], pst[:], AF.Sqrt, [t_pst], [t_rt], scale=1.0 / D, bias=EPS)
            RCP(rstd[:], rt[:], [t_rt], [t_rstd])
            for kc in range(8):
                b = kc % 2
                STT(tmp[b][:], xT[:, kc, tok(tt)], AG[:, 2 * k, kc:kc + 1], rstd[:], ALU.mult, ALU.mult,
                    [xT_T[kc][tt], t_rstd, tAG], [t_tmp[b]])
                ACTV(dst(kc), tmp[b][:], AF.Identity, [t_tmp[b], tMod], [t_dst(kc)],
                     bias=modT[:, 3 * k * 8 + kc:3 * k * 8 + kc + 1])

        def norm_bufs(Ep, tag):
            sq = Ep(nc.sbuf_tensor(tag + "sq", [128, 8, 512], BF16))
            rt = Ep(nc.sbuf_tensor(tag + "rt", [128, 512], F32))
            rstd = Ep(nc.sbuf_tensor(tag + "rstd", [128, 512], F32))
            tmp = [Ep(nc.sbuf_tensor(tag + "tmp%d" % i, [128, 512], F32)) for i in range(2)]
            pst = Ep(nc.psum_tensor(tag + "pst", [128, 512], F32))
            return (sq, rt, rstd, tmp, pst, T(), T(), T(), Ts(2), T())

        tXK, tAGK, tXV, tAGV = Ts(NKC), Ts(NKC), Ts(NVC), Ts(NVC)
        tXL, tAGL = Ts(2)
        tQAd, tQFd, tQId, tWd, tFKd, tFQd, tOTd, tXTd = Ts(8)

        def phase3():
            with ExitStack() as ph:
                Ep = ph.enter_context
                h2T = Ep(nc.sbuf_tensor("p3h2T", [128, 8, NT], BF16))
                nb = norm_bufs(Ep, "p3")
                wt = [Ep(nc.sbuf_tensor("p3wt%d" % i, [128, 8, 512], BF16)) for i in range(2)]
                gn = Ep(nc.sbuf_tensor("p3gn", [128, 4], F32))
                gq = Ep(nc.sbuf_tensor("p3gq", [128, 4], F32))
                onesbd = Ep(nc.sbuf_tensor("p3onesbd", [128, 128], BF16))
                bfT = Ep(nc.sbuf_tensor("p3bf", [8, 1], F32))
                nbf = Ep(nc.sbuf_tensor("p3nbf", [8, 1], F32))
                sqh = [Ep(nc.sbuf_tensor("p3sqh%d" % i, [128, 512], BF16)) for i in range(2)]
                rth = [Ep(nc.sbuf_tensor("p3rth%d" % i, [128, 512], F32)) for i in range(2)]
                rsh = [Ep(nc.sbuf_tensor("p3rsh%d" % i, [128, 512], F32)) for i in range(2)]
                nzs = [Ep(nc.sbuf_tensor("p3nzs%d" % i, [128, NT], BF16)) for i in range(2)]
                vbig = [Ep(nc.sbuf_tensor("p3vbig%d" % i, [128, 8, 16, 64], BF16)) for i in range(2)]
                wst = Ep(nc.sbuf_tensor("p3wst", [8, NT], F32))
                lst = Ep(nc.sbuf_tensor("p3lst", [8, NT], F32))
                let_ = Ep(nc.sbuf_tensor("p3let", [8, 512], F32))
                psz = [Ep(nc.psum_tensor("p3psz%d" % i, [128, 512], F32)) for i in range(2)]
                pss = [Ep(nc.psum_tensor("p3pss%d" % i, [128, 512], F32)) for i in range(2)]
                psv = [Ep(nc.psum_tensor("p3psv%d" % i, [128, 512], F32)) for i in range(2)]
                block = Ep(nc.Block())
                t_h2 = [Ts(4) for _ in range(8)]
                t_wt, t_sqh, t_rth, t_rsh, t_nzs, t_vst, t_psz, t_pss, t_psv = (Ts(2) for _ in range(9))
                t_gn, t_gq, t_bf, t_nbf, t_wst, t_lst, t_let = Ts(7)
                DMA(gn[:], gn_d, [], [t_gn])
                DMA(onesbd[:], onesbd_d, [], [t_gn])
                DMA(bfT[:], bfT_d, [], [t_bf])
                TS(gq[:], gn[:], 0.125, None, ALU.mult, None, [t_gn], [t_gq])
                TS(nbf[:], bfT[:], -1.0, None, ALU.mult, None, [t_bf], [t_nbf])
                for h in range(8):
                    DMA(XKc[h].ap()[64:66, :], posK_d, [], [tXK[h]])
                for tt in range(4):
                    emit_norm(1, tt, nb, lambda kc, tt=tt: h2T[:, kc, tok(tt)], lambda kc, tt=tt: t_h2[kc][tt])
                cnt = {"w": 0, "p": 0, "s": 0, "v": 0}

                wgroups = [(3584, 80), (512, 512), (2048, 512), (1024, 512), (2560, 512), (0, 512), (1536, 512), (3072, 512)]
                wstate = {"next": 0}

                def issue_w():
                    i = wstate["next"]
                    if i < len(wgroups):
                        c0_, nc_ = wgroups[i]
                        DMA(wt[i % 2][:, :, 0:nc_], w_in[:, c0_:c0_ + nc_].rearrange("(kc p) f -> p kc f", p=128), [], [t_wt[i % 2]], q="pool")
                        wstate["next"] = i + 1

                def load_w(c0, ncols):
                    i = cnt["w"]
                    assert wgroups[i] == (c0, ncols)
                    cnt["w"] += 1
                    if wstate["next"] <= i:
                        issue_w()
                    issue_w()
                    return i % 2

                def gather(ci_list, kind):
                    for ci in ci_list:
                        if kind == "k":
                            src, dst, t_s, t_d = XKc[ci], AGKc[ci], tXK[ci], tAGK[ci]
                        elif kind == "v":
                            src, dst, t_s, t_d = XVc[ci], AGVc[ci], tXV[ci], tAGV[ci]
                        else:
                            src, dst, t_s, t_d = XL, AGL, tXL, tAGL
                        P.coll(lambda e, src=src, dst=dst: e.collective_compute(
                            "AllGather", ALU.bypass, replica_groups=RG, ins=[src.ap().opt()], outs=[dst.ap().opt()]),
                            reads=[t_s], writes=[t_d])

                def proj(b, off, M, tt):
                    pb = cnt["p"] % 2
                    cnt["p"] += 1
                    for kc in range(8):
                        MM(psz[pb][0:M, :], wt[b][:, kc, off:off + M], h2T[:, kc, tok(tt)], kc == 0, kc == 7,
                           [t_wt[b], t_h2[kc][tt]], [t_psz[pb]])
                    return pb

                def qk_group(c0, gcol, t_g, dst_fn, t_dst, after=None):
                    b = load_w(c0, 512)
                    for hp in range(4):
                        sb = cnt["s"] % 2
                        cnt["s"] += 1
                        for tt in range(4):
                            pb = proj(b, hp * 128, 128, tt)
                            ACTV(sqh[pb][:], psz[pb][:], AF.Square, [t_psz[pb]], [t_sqh[pb]])
                            MM(pss[pb][:], onesbd[:], sqh[pb][:], True, True, [t_sqh[pb], t_gn], [t_pss[pb]])
                            ACTV(rth[pb][:], pss[pb][:], AF.Sqrt, [t_pss[pb]], [t_rth[pb]], scale=1.0 / 64, bias=EPS)
                            RCP(rsh[pb][:], rth[pb][:], [t_rth[pb]], [t_rsh[pb]])
                            STT(nzs[sb][:, tok(tt)], psz[pb][:], gcol, rsh[pb][:], ALU.mult, ALU.mult,
                                [t_psz[pb], t_rsh[pb], t_g], [t_nzs[sb]])
                        for hl in range(2):
                            h = 2 * hp + hl
                            DMA(dst_fn(h), nzs[sb][hl * 64:(hl + 1) * 64, :], [t_nzs[sb]], [t_dst(h)])
                            if after is not None:
                                after(h)

                b = load_w(3584, 80)
                sb = cnt["s"] % 2
                cnt["s"] += 1
                for tt in range(4):
                    pb = proj(b, 0, 64, tt)
                    COPY(nzs[sb][0:64, tok(tt)], psz[pb][0:64, :], [t_psz[pb]], [t_nzs[sb]])
                DMA(XKc[12].ap()[0:64, :], nzs[sb][0:64, :], [t_nzs[sb]], [tXK[12]])
                gather([12], "k")
                for tt in range(4):
                    pb = proj(b, 64, 8, tt)
                    COPY(wst[:, tok(tt)], psz[pb][0:8, :], [t_psz[pb]], [t_wst])
                DMA(Wd.ap(), wst[:], [t_wst], [tWd])
                for tt in range(4):
                    pb = proj(b, 72, 8, tt)
                    ACTV(let_[:], psz[pb][0:8, :], AF.Exp, [t_psz[pb], t_nbf], [t_let], scale=-1.0, bias=nbf[:, 0:1])
                    ACTV(let_[:], let_[:], AF.Ln, [t_let], [t_let], scale=1.0, bias=1.0)
                    TS(lst[:, tok(tt)], let_[:], -1.0, None, ALU.mult, None, [t_let], [t_lst])
                DMA(XL.ap(), lst[:], [t_lst], [tXL])
                gather([0], "l")
                qk_group(512, gn[:, 1:2], t_gn, lambda h: XKc[h].ap()[0:64, :], lambda h: tXK[h],
                         after=lambda h: gather([h], "k"))
                qk_group(2048, gn[:, 3:4], t_gn, lambda h: XKc[8 + h // 2].ap()[(h % 2) * 64:(h % 2) * 64 + 64, :],
                         lambda h: tXK[8 + h // 2], after=lambda h: (gather([8 + h // 2], "k") if h % 2 == 1 else None))

                XV4 = [XVc[i].ap().rearrange("p (hh m d) -> p hh m d", hh=2, m=16) for i in range(NVC)]
                for vi, c0 in enumerate((1024, 2560)):
                    b = load_w(c0, 512)
                    for mt in range(16):
                        pb = cnt["v"] % 2
                        cnt["v"] += 1
                        for kc in range(8):
                            MM(psv[pb][:], h2T[:, kc, mt * 128:(mt + 1) * 128], wt[b][:, kc, :], kc == 0, kc == 7,
                               [t_wt[b], t_h2[kc][mt // 4]], [t_psv[pb]])
                        COPY(vbig[vi][:, :, mt, :], psv[pb][:, :].rearrange("p (h d) -> p h d", h=8), [t_psv[pb]], [t_vst[vi]])
                    for cc in range(4):
                        DMA(XVc[vi * 4 + cc].ap(), vbig[vi][:, 2 * cc:2 * cc + 2, :, :].rearrange("p h m d -> p (h m d)"),
                            [t_vst[vi]], [tXV[vi * 4 + cc]])
                    gather([vi * 4 + cc for cc in range(4)], "v")

                qk_group(0, gq[:, 0:1], t_gq, lambda h: QAd.ap()[h], lambda h: tQAd)
                qk_group(1536, gq[:, 2:3], t_gq, lambda h: QFd.ap()[h], lambda h: tQFd)
                b = load_w(3072, 512)
                for ip in range(4):
                    sb = cnt["s"] % 2
                    cnt["s"] += 1
                    for tt in range(4):
                        pb = proj(b, ip * 128, 128, tt)
                        COPY(nzs[sb][:, tok(tt)], psz[pb][:], [t_psz[pb]], [t_nzs[sb]])
                    for il in range(2):
                        DMA(QId.ap()[:, 2 * ip + il, :], nzs[sb][il * 64:(il + 1) * 64, :], [t_nzs[sb]], [tQId])
                DMA(XTd.ap(), xT[:, :, :].rearrange("p a t -> p (a t)"),
                    [xT_T[fc][tt] for fc in range(8) for tt in range(4)], [tXTd])
                P.barrier()
                P.flush(block)

        def phase35():
            with ExitStack() as ph:
                Ep = ph.enter_context
                W = 4096
                lf = Ep(nc.sbuf_tensor("p35lf", [8, W], F32))
                Fc = Ep(nc.sbuf_tensor("p35F", [8, W], F32))
                Fo = Ep(nc.sbuf_tensor("p35Fo", [8, 1024], F32))
                ones = Ep(nc.sbuf_tensor("p35ones", [8, W], F32))
                carry = Ep(nc.sbuf_tensor("p35carry", [8, 1], F32))
                selj = Ep(nc.sbuf_tensor("p35selj", [8, 4], F32))
                r1 = Ep(nc.sbuf_tensor("p35r1", [8, W], F32))
                hb = [Ep(nc.sbuf_tensor("p35hb%d" % i, [8, W], BF16)) for i in range(3)]
                nhb = [Ep(nc.sbuf_tensor("p35nhb%d" % i, [8, W], BF16)) for i in range(3)]
                r1o = Ep(nc.sbuf_tensor("p35r1o", [8, 1024], F32))
                hbo = [Ep(nc.sbuf_tensor("p35hbo%d" % i, [8, 1024], BF16)) for i in range(3)]
                block = Ep(nc.Block())
                t_lf, t_F, t_Fo, t_ones, t_carry, t_selj, t_r1, t_r1o = Ts(8)
                t_hb, t_nhb, t_hbo = Ts(3), Ts(3), Ts(3)
                MSET(ones[:], 1.0, [t_ones])
                DMA(selj[:], seljF_d, [], [t_selj])

                def split(src, t_src, n, r, t_r, pieces, t_pieces):
                    TC(pieces[0][:, 0:n], src[:, 0:n], [t_src], [t_pieces[0]])
                    TT(r[:, 0:n], src[:, 0:n], pieces[0][:, 0:n], ALU.subtract, [t_src, t_pieces[0]], [t_r])
                    TC(pieces[1][:, 0:n], r[:, 0:n], [t_r], [t_pieces[1]])
                    TT(src[:, 0:n], r[:, 0:n], pieces[1][:, 0:n], ALU.subtract, [t_r, t_pieces[1]], [t_src])
                    TC(pieces[2][:, 0:n], src[:, 0:n], [t_src], [t_pieces[2]])

                lf4 = lf[:, :].rearrange("h (m j p) -> h m j p", m=8, j=4)
                F4 = Fc[:, :].rearrange("h (m j p) -> h m j p", m=8, j=4)
                Fo3 = Fo[:, :].rearrange("h (m p) -> h m p", m=8)
                for q in range(2):
                    for j in range(4):
                        DMA(lf4[:, :, j, :], AGL.ap()[j * 8:(j + 1) * 8, q * 1024:(q + 1) * 1024].rearrange("h (m p) -> h m p", p=128),
                            [tAGL], [t_lf])
                    init = 0.0 if q == 0 else carry[:, 0:1]
                    P.op("dve", lambda e, init=init: e.tensor_tensor_scan(out=Fc[:], data0=ones[:], data1=lf[:], initial=init,
                                                                          op0=ALU.mult, op1=ALU.add),
                         reads=[t_ones, t_lf, t_carry], writes=[t_F])
                    TC(carry[:], Fc[:, W - 1:W], [t_F], [t_carry])
                    TS(Fo3, F4[:, :, 0, :], selj[:, 0:1], None, ALU.mult, None, [t_F, t_selj], [t_Fo])
                    for j in range(1, 4):
                        STT(Fo3, F4[:, :, j, :], selj[:, j:j + 1], Fo3, ALU.mult, ALU.add, [t_F, t_selj, t_Fo], [t_Fo])
                    split(Fo, t_Fo, 1024, r1o, t_r1o, hbo, t_hbo)
                    for r in range(3):
                        DMA(FQd.ap()[:, r, q * 1024:(q + 1) * 1024], hbo[r][:], [t_hbo[r]], [tFQd])
                    split(Fc, t_F, W, r1, t_r1, hb, t_hb)
                    for r in range(3):
                        TS(nhb[r][:], hb[r][:], -1.0, None, ALU.mult, None, [t_hb[r]], [t_nhb[r]])
                        nh4 = nhb[r][:, :].rearrange("h (m j p) -> h m j p", m=8, j=4)
                        for j in range(4):
                            DMA(FKd.ap()[:, r, j, q * 1024:(q + 1) * 1024].rearrange("h (m p) -> h m p", p=128),
                                nh4[:, :, j, :], [t_nhb[r]], [tFKd])
                P.barrier()
                P.flush(block)

        def phase4():
            FP8 = mybir.dt.float8e4
            with ExitStack() as ph:
                Ep = ph.enter_context
                scores2 = [Ep(nc.sbuf_tensor("p4sc%d" % i, [128, S], F32)) for i in range(2)]
                maskb1 = Ep(nc.sbuf_tensor("p4mb", [128, S], BF16))
                maskb2 = [maskb1, maskb1]
                cntj = Ep(nc.sbuf_tensor("p4cntj", [128, S], FP8))
                maskT = Ep(nc.sbuf_tensor("p4mT", [128, 64, 512], FP8))
                kidx = Ep(nc.sbuf_tensor("p4kidx", [128, 8, 512], BF16))
                QIb1 = Ep(nc.sbuf_tensor("p4qi", [128, 8, 128], BF16))
                QIb = [QIb1, QIb1]
                QIg = [Ep(nc.sbuf_tensor("p4qg%d" % i, [128, 8, 128], BF16)) for i in range(2)]
                QAt = Ep(nc.sbuf_tensor("p4qa", [68, 8, 512], BF16))
                QFt = Ep(nc.sbuf_tensor("p4qf", [70, 8, 512], BF16))
                NKB = 2
                NPT = 4
                LAG = 3
                NVB = 4
                KAb = [Ep(nc.sbuf_tensor("p4ka%d" % i, [68, 4, 512], BF16)) for i in range(NKB)]
                KFb = [Ep(nc.sbuf_tensor("p4kf%d" % i, [70, 4, 512], BF16)) for i in range(NKB)]
                Vb = [Ep(nc.sbuf_tensor("p4v%d" % i, [128, 4, 4, 65], BF16)) for i in range(NVB)]
                Rsb = [Ep(nc.sbuf_tensor("p4rs%d" % i, [128, 512], BF16)) for i in range(3)]
                PT = [Ep(nc.sbuf_tensor("p4pt%d" % i, [128, 512], BF16)) for i in range(NPT)]
                on = [Ep(nc.sbuf_tensor("p4on%d" % i, [65, 512], F32)) for i in range(2)]
                rc = Ep(nc.sbuf_tensor("p4rc", [65, 512], F32))
                onesf = Ep(nc.sbuf_tensor("p4onesf", [65, 64], F32))
                OTs = [Ep(nc.sbuf_tensor("p4ots%d" % i, [64, 512], BF16)) for i in range(2)]
                wcol = Ep(nc.sbuf_tensor("p4wcol", [128, 16, 8], F32))
                sel = Ep(nc.sbuf_tensor("p4sel", [128, 8, 128], BF16))
                identb = Ep(nc.sbuf_tensor("p4idb", [128, 128], BF16))
                identS = Ep(nc.sbuf_tensor("p4idS", [128, 128], BF16))
                cmaskf = Ep(nc.sbuf_tensor("p4cmf", [128, 512], F32))
                cmT = Ep(nc.sbuf_tensor("p4cmT", [128, 4, 128], BF16))
                posrow = Ep(nc.sbuf_tensor("p4pos", [128, 512], F32))
                pow2 = Ep(nc.sbuf_tensor("p4pow2", [128, NIT], F32))
                junk = Ep(nc.sbuf_tensor("p4junk", [128, 512], F32))
                bs = Ep(nc.sbuf_tensor("p4bs", [128, 16], F32))
                Hh = Ep(nc.sbuf_tensor("p4H", [128, NIT], F32))
                U = Ep(nc.sbuf_tensor("p4U", [128, 4], F32))
                sbf = Ep(nc.sbuf_tensor("p4sbf", [128, 2], BF16))
                bank = [Ep(nc.psum_tensor("p4bank%d" % i, [128, 512], F32)) for i in range(7)]
                tpb = Ep(nc.psum_tensor("p4tpb", [128, 1024], BF16))
                t_bank = Ts(7)
                t_tpb = T()
                Rps, t_Rps = bank[0:2], t_bank[0:2]
                scps, t_scps = bank[2:4], t_bank[2:4]
                sps, t_sps = bank[0:4], t_bank[0:4]
                ops, t_ops = bank[4:6], t_bank[4:6]
                bcps, t_bcps = bank[6], t_bank[6]
                block = Ep(nc.Block())
                t_sc2 = [Ts(16), Ts(16)]
                t_mb1 = T()
                t_mb2 = [t_mb1, t_mb1]
                t_cntj = T()
                t_mT = T()
                t_kidx, t_wcol, t_cst, t_bs, t_H, t_U, t_sbf, t_junk, t_rc, t_onesf = Ts(10)
                t_QI1 = T()
                t_QI, t_QG, t_on, t_OTs = [t_QI1, t_QI1], Ts(2), Ts(2), Ts(2)
                t_QA, t_QF = T(), T()
                t_KA, t_KF, t_V = Ts(NKB), Ts(NKB), Ts(NVB)
                t_Rsb, t_PT = Ts(3), Ts(NPT)
                AGK3 = [AGKc[i].ap().rearrange("(j r) c -> j r c", j=4) for i in range(NKC)]
                AGV5 = [AGVc[i].ap().rearrange("(j p) (hh m d) -> j p hh m d", j=4, hh=2, m=16) for i in range(NVC)]
                for (dst, src) in ((sel, sel_d), (identb, identb_d), (cmaskf, cmaskf_d), (cmT, cmT_d),
                                   (posrow, posrow_d), (pow2, pow2_d)):
                    DMA(dst[:], src, [], [t_cst])
                for j in range(4):
                    for par in range(2):
                        DMA(kidx[par * 64:(par + 1) * 64, :, j * 128:(j + 1) * 128],
                            AGK3[12][j, 0:64, :].rearrange("r (s two p) -> r s two p", two=2, p=128)[:, :, par, :], [tAGK[12]], [t_kidx])
                for i in range(8):
                    DMA(wcol[i * 16:(i + 1) * 16, :, :], Wd.ap()[i, :].rearrange("(m g t) -> t m g", g=8, t=16),
                        [tWd], [t_wcol], allow_slow_non_contiguous=True)
                for i in range(NKB):
                    MSET(KAb[i][64:68, :, :], 1.0, [t_KA[i]])
                    MSET(KFb[i][64:70, :, :], 1.0, [t_KF[i]])
                for i in range(NVB):
                    MSET(Vb[i][:, :, :, :], 1.0, [t_V[i]])
                MSET(QFt[64:70, :, :], 1.0, [t_QF])
                MSET(U[:], 1.0, [t_U])
                MSET(onesf[:], 1.0, [t_onesf])
                t_idS = T()
                wabs = Ep(nc.sbuf_tensor("p4wabs", [128, 16, 8], F32))
                wsgn = Ep(nc.sbuf_tensor("p4wsgn", [128, 16, 8], F32))
                Sels = [Ep(nc.sbuf_tensor("p4sels%d" % i, [128, 8, 128], BF16)) for i in range(2)]
                t_wabs, t_wsgn = T(), T()
                t_Sels = Ts(2)
                TS(wsgn[:], wcol[:], 0.0, 2.0, ALU.is_ge, ALU.mult, [t_wcol], [t_wsgn])
                TS(wsgn[:], wsgn[:], -1.0, None, ALU.add, None, [t_wsgn], [t_wsgn])
                TT(wabs[:], wcol[:], wsgn[:], ALU.mult, [t_wcol, t_wsgn], [t_wabs])
                TS(identS[:], identb[:], 128.0, None, ALU.mult, None, [t_cst], [t_idS])
                cn = {"k": 0, "v": 0, "r": 0, "pt": 0, "sp": 0, "op": 0, "sc": 0, "on": 0}

                def load_qbatch(m0):
                    tsl = slice(m0 * 128, m0 * 128 + 512)
                    DMA(QAt[0:64, :, :], QAd.ap()[:, :, tsl].rearrange("h d t -> d h t"), [tQAd], [t_QA])
                    DMA(QFt[0:64, :, :], QFd.ap()[:, :, tsl].rearrange("h d t -> d h t"), [tQFd], [t_QF])
                    DMA(QFt[67:70, :, :], FQd.ap()[:, :, tsl].rearrange("h r t -> r h t"), [tFQd], [t_QF])

                def load_qi(m):
                    qb = m % 2
                    for par in range(2):
                        DMA(QIb[qb][par * 64:(par + 1) * 64, :, :], QId.ap()[:, :, m * 128:(m + 1) * 128], [tQId], [t_QI[qb]])
                    qsrc = QIb[qb][:, :, :].rearrange("d i (g t) -> d g i t", t=16)
                    qdst = QIg[qb][:, :, :].rearrange("d g (i t) -> d g i t", t=16)
                    P.op("pool", lambda e: e.tensor_copy(out=qdst, in_=qsrc), [t_QI[qb]], [t_QG[qb]])
                    for g in range(8):
                        so, si, ss = Sels[qb][:, g, :], sel[:, g, :], wsgn[:, m, g:g + 1]
                        P.op("pool", lambda e, so=so, si=si, ss=ss: e.tensor_scalar(out=so, in0=si, scalar1=ss, scalar2=None, op0=ALU.mult),
                             [t_cst, t_wsgn], [t_Sels[qb]])

                def idx(m, sp):
                    qb = m % 2
                    scores, t_sc = scores2[sp], t_sc2[sp]
                    for mp in range(m + 1):
                        sc = cn["sc"] % 2
                        cn["sc"] += 1
                        p0 = (mp % 2) * 64
                        rhs_k = kidx[p0:p0 + 64, mp // 2, :]

                        def emit_R(g, p0=p0, rhs_k=rhs_k):
                            MM(Rps[g % 2][:], QIg[qb][p0:p0 + 64, g, :], rhs_k, True, True, [t_QG[qb], t_kidx], [t_Rps[g % 2]])
                        emit_R(0)
                        for g in range(8):
                            rb = g % 2
                            rs = cn["r"] % 3
                            cn["r"] += 1
                            ACTV(Rsb[rs][:], Rps[rb][:], AF.Relu, [t_Rps[rb], t_wabs], [t_Rsb[rs]], scale=wabs[:, m, g:g + 1])
                            if g < 7:
                                emit_R(g + 1)
                            MM(scps[sc][:], Sels[qb][:, g, :], Rsb[rs][:], g == 0, g == 7, [t_Rsb[rs], t_Sels[qb]], [t_scps[sc]])
                        csl = slice(mp * 512, (mp + 1) * 512)
                        if mp < m:
                            ACTV(scores[:, csl], scps[sc][:], AF.Copy, [t_scps[sc]], [t_sc[mp]])
                        else:
                            RED(bs[:, 5:6], scps[sc][:], ALU.min, [t_scps[sc]], [t_bs])
                            TT(scores[:, csl], scps[sc][:], cmaskf[:], ALU.add, [t_scps[sc], t_cst], [t_sc[mp]])

                def thresh_steps(m, sp):
                    L = 512 * (m + 1)
                    scores, t_sc, maskb, t_mb = scores2[sp], t_sc2[sp], maskb2[sp], t_mb2[sp]
                    rd = [t_sc[i] for i in range(m + 1)]
                    steps = []

                    def s_init():
                        RED(bs[:, 1:2], scores[:, 0:L], ALU.max, rd, [t_bs])
                        if m > 0:
                            RED(bs[:, 0:1], scores[:, 0:L - 512], ALU.min, rd, [t_bs])
                            TT(bs[:, 0:1], bs[:, 0:1], bs[:, 5:6], ALU.min, [t_bs], [t_bs])
                        else:
                            TC(bs[:, 0:1], bs[:, 5:6], [t_bs], [t_bs])
                        TT(bs[:, 4:5], bs[:, 1:2], bs[:, 0:1], ALU.subtract, [t_bs], [t_bs])
                        TS(Hh[:], pow2[:], bs[:, 4:5], None, ALU.mult, None, [t_bs, t_cst], [t_H])
                    steps.append(s_init)

                    def mk_iter(k):
                        def s_iter():
                            TT(bs[:, 2:3], bs[:, 0:1], Hh[:, k:k + 1], ALU.add, [t_bs, t_H], [t_bs])
                            P.op("dve", lambda e: e.tensor_scalar(out=cntj[:, 0:L], in0=scores[:, 0:L], scalar1=bs[:, 2:3], scalar2=None,
                                                                  op0=ALU.is_ge, op1=ALU.add, accum_out=bs[:, 3:4], saturate=False),
                                 rd + [t_bs], [t_cntj, t_bs])
                            TS(bs[:, 4:5], bs[:, 3:4], float(TOPK) - 0.5, Hh[:, k:k + 1], ALU.is_ge, ALU.mult, [t_bs, t_H], [t_bs])
                            TT(bs[:, 0:1], bs[:, 0:1], bs[:, 4:5], ALU.add, [t_bs], [t_bs])
                        return s_iter
                    for k in range(NIT):
                        steps.append(mk_iter(k))

                    def s_fin():
                        TS(maskb[:, 0:L], scores[:, 0:L], bs[:, 0:1], NEGA, ALU.is_lt, ALU.mult, rd + [t_bs], [t_mb])
                        for c in range(m + 1):
                            STT(junk[:], maskb[:, c * 512:(c + 1) * 512], 200.0, posrow[:], ALU.mult, ALU.add, [t_mb, t_cst], [t_junk])
                            RED(bs[:, 6:7], junk[:], ALU.max, [t_junk], [t_bs])
                            if c == 0:
                                TC(bs[:, 7:8], bs[:, 6:7], [t_bs], [t_bs])
                            else:
                                STT(bs[:, 7:8], bs[:, 6:7], float(512 * c), bs[:, 7:8], ALU.add, ALU.max, [t_bs], [t_bs])
                        TC(sbf[:, 0:1], bs[:, 7:8], [t_bs], [t_sbf])
                        TS(U[:, 2:3], sbf[:, 0:1], -1.0, None, ALU.mult, None, [t_sbf], [t_U])
                        TT(U[:, 3:4], sbf[:, 0:1], bs[:, 7:8], ALU.subtract, [t_sbf, t_bs], [t_U])
                    steps.append(s_fin)
                    return steps

                def run_steps(steps, n=None):
                    k = 0
                    while steps and (n is None or k < n):
                        steps.pop(0)()
                        k += 1

                def thresh_pe(m, q, sp):
                    maskb, t_mb = maskb2[sp], t_mb2[sp]
                    sc = cn["sc"] % 2
                    cn["sc"] += 1
                    TR(scps[sc][0:4, 0:128], U[:, 0:4], identf[:], [t_U, tConst], [t_scps[sc]])
                    for h in range(8):
                        ACTV(QAt[64:68, h, q * 128:(q + 1) * 128], scps[sc][0:4, 0:128], AF.Copy, [t_scps[sc]], [t_QA], scale=2.0 ** -(h + 1))
                    for mp in range(m + 1):
                        for jp in range(4):
                            col = mp * 512 + jp * 128
                            TR(tpb[:, jp * 128:(jp + 1) * 128], maskb[:, col:col + 128], identb[:], [t_mb, t_cst], [t_tpb])
                        o_ = maskT[:, 4 * mp:4 * mp + 4, q * 128:(q + 1) * 128]
                        i_ = tpb[:, 0:512].rearrange("p (j t) -> p j t", j=4)
                        P.op("act", lambda e, o_=o_, i_=i_: e.activation(out=o_, in_=i_, func=AF.Copy, saturate=False), [t_tpb], [t_mT])

                pend_fin = []

                def flush_fin():
                    while pend_fin:
                        oi, idx16, m0 = pend_fin.pop(0)
                        RCP(rc[64:65, :], on[oi][64:65, :], [t_on[oi]], [t_rc])
                        MM(bcps[0:64, :], onesf[64:65, 0:64], rc[64:65, :], True, True, [t_rc, t_onesf], [t_bcps])
                        ot = idx16 % 2
                        TT(OTs[ot][:], on[oi][0:64, :], bcps[0:64, :], ALU.mult, [t_on[oi], t_bcps], [t_OTs[ot]])
                        DMA(OTd.ap()[idx16, :, m0 * 128:m0 * 128 + 512], OTs[ot][:], [t_OTs[ot]], [tOTd])

                def emit_pv(pt, vb, jp, mpl, c0, first, last, ob, idx16, m0):
                    MM(ops[ob][0:65, c0:512], Vb[vb][:, jp, mpl, :], PT[pt][:, c0:512], first, last, [t_PT[pt], t_V[vb]], [t_ops[ob]])
                    if last:
                        oi = cn["on"] % 2
                        cn["on"] += 1
                        ACTV(on[oi][:], ops[ob][0:65, :], AF.Copy, [t_ops[ob]], [t_on[oi]])
                        pend_fin.append((oi, idx16, m0))

                def attn(mixer, m0, heads, fin_now, inter=None):
                    isA = (mixer == 0)
                    Qt = QAt if isA else QFt
                    t_Q = t_QA if isA else t_QF
                    R = 68 if isA else 70
                    pend = []
                    for h in heads:
                        if fin_now:
                            flush_fin()
                        if inter:
                            run_steps(inter, 3)
                        ob = cn["op"] % 2
                        cn["op"] += 1
                        kb = vb = None
                        for mp in range(m0 + 4):
                            mpl = mp % 4
                            c0 = max(0, mp - m0) * 128
                            if mpl == 0:
                                nm = min(4, m0 + 4 - mp)
                                w = nm * 128
                                kb = cn["k"] % NKB
                                cn["k"] += 1
                                vb = cn["v"] % NVB
                                cn["v"] += 1
                                k0 = mp * 128
                                if isA:
                                    DMA(KAb[kb][0:66, :, 0:w], AGK3[h][:, 0:66, k0:k0 + w].rearrange("j r c -> r j c"),
                                        [tAGK[h]], [t_KA[kb]])
                                else:
                                    DMA(KFb[kb][0:64, :, 0:w],
                                        AGK3[8 + h // 2][:, (h % 2) * 64:(h % 2) * 64 + 64, k0:k0 + w].rearrange("j r c -> r j c"),
                                        [tAGK[8 + h // 2]], [t_KF[kb]])
                                    DMA(KFb[kb][64:67, :, 0:w], FKd.ap()[h, :, :, k0:k0 + w], [tFKd], [t_KF[kb]])
                                hh = h if isA else 8 + h
                                for jj in range(4):
                                    DMA(Vb[vb][:, jj, 0:nm, 0:64], AGV5[hh // 2][jj, :, hh % 2, mp:mp + nm, :],
                                        [tAGV[hh // 2]], [t_V[vb]])
                            Kt = KAb[kb] if isA else KFb[kb]
                            t_K = t_KA[kb] if isA else t_KF[kb]
                            for jp in range(4):
                                sb_ = cn["sp"] % 4
                                cn["sp"] += 1
                                diag = (not isA) and (mp >= m0)
                                MM(sps[sb_][:, c0:512], Kt[0:R, jp, mpl * 128:(mpl + 1) * 128], Qt[0:R, h, c0:512], True, not (isA or diag),
                                   [t_K, t_Q], [t_sps[sb_]])
                                if isA:
                                    MM(sps[sb_][:, c0:512], identS[:], maskT[:, 4 * mp + jp, c0:512], False, True, [t_mT, t_idS], [t_sps[sb_]])
                                elif diag:
                                    MM(sps[sb_][:, c0:c0 + 128], identb[:], cmT[:, jp, :], False, True, [t_cst], [t_sps[sb_]])
                                pt = cn["pt"] % NPT
                                cn["pt"] += 1
                                ACTV(PT[pt][:, c0:512], sps[sb_][:, c0:512], AF.Exp, [t_sps[sb_]], [t_PT[pt]])
                                first = (mp == 0 and jp == 0)
                                last = (mp == m0 + 3 and jp == 3)
                                pend.append((pt, vb, jp, mpl, c0, first, last, ob, mixer * 8 + h, m0))
                                if len(pend) > LAG:
                                    emit_pv(*pend.pop(0))
                    while pend:
                        emit_pv(*pend.pop(0))

                seq = [4 * B + q for B in range(4) for q in (3, 2, 1, 0)]
                load_qi(seq[0])
                idx(seq[0], 0)
                cur = thresh_steps(seq[0], 0)
                for k, m in enumerate(seq):
                    B, q = divmod(m, 4)
                    m0 = 4 * B
                    sp = k % 2
                    if q == 3:
                        load_qbatch(m0)
                    flush_fin()
                    run_steps(cur)
                    if k + 1 < 16:
                        load_qi(seq[k + 1])
                        idx(seq[k + 1], (k + 1) % 2)
                    attn(1, m0, [2 * (3 - q), 2 * (3 - q) + 1], False)
                    thresh_pe(m, q, sp)
                    cur = thresh_steps(seq[k + 1], (k + 1) % 2) if k + 1 < 16 else []
                    if q == 0:
                        flush_fin()
                        attn(0, m0, list(range(8)), True, inter=cur)
                        flush_fin()
                P.barrier()
                P.flush(block)

        def phase5():
            with ExitStack() as ph:
                Ep = ph.enter_context
                nb = norm_bufs(Ep, "p5")
                h2 = Ep(nc.sbuf_tensor("p5h2", [128, 8, 512], BF16))
                OT = Ep(nc.sbuf_tensor("p5OT", [128, 8, 512], BF16))
                mg = Ep(nc.sbuf_tensor("p5mg", [128, 8, 512], BF16))
                wga = Ep(nc.sbuf_tensor("p5wga", [128, 8, D], BF16))
                wgb_ = Ep(nc.sbuf_tensor("p5wgb", [128, 8, D], BF16))
                wbA = Ep(nc.sbuf_tensor("p5wbA", [128, 4, D], BF16))
                wbF = Ep(nc.sbuf_tensor("p5wbF", [128, 4, D], BF16))
                wo = Ep(nc.sbuf_tensor("p5wo", [128, 8, D], BF16))
                identb = Ep(nc.sbuf_tensor("p5idb", [128, 128], BF16))
                Ot = [Ep(nc.sbuf_tensor("p5Ot%d" % i, [128, D], BF16)) for i in range(2)]
                sA = Ep(nc.sbuf_tensor("p5sA", [128, 512], F32))
                sB = Ep(nc.sbuf_tensor("p5sB", [128, 512], F32))
                t1 = Ep(nc.sbuf_tensor("p5t1", [128, 512], F32))
                t2 = Ep(nc.sbuf_tensor("p5t2", [128, 512], F32))
                psA = Ep(nc.psum_tensor("p5psA", [128, 512], F32))
                psB = Ep(nc.psum_tensor("p5psB", [128, 512], F32))
                psyA = Ep(nc.psum_tensor("p5psyA", [128, 512], F32))
                psyB = Ep(nc.psum_tensor("p5psyB", [128, 512], F32))
                pso = Ep(nc.psum_tensor("p5pso", [128, 512], F32))
                ptr = Ep(nc.psum_tensor("p5ptr", [128, 1024], BF16))
                block = Ep(nc.Block())
                t_w, t_idb, t_sA, t_sB, t_t1, t_t2, t_psA, t_psB, t_psyA, t_psyB, t_pso, t_ptr = Ts(12)
                t_h2, t_OT, t_mg, t_Ot = Ts(8), Ts(8), Ts(8), Ts(2)
                DMA(xT[:, :, :].rearrange("p a t -> p (a t)"), XTd.ap(), [tXTd], [xT_T[fc][tt] for fc in range(8) for tt in range(4)])
                DMA(identb[:], identb_d, [], [t_idb])
                DMA(wga[:], w_in[:, 3664:4688].rearrange("(kc p) f -> p kc f", p=128), [], [t_w], q="pool")
                DMA(wgb_[:], w_in[:, 4688:5712].rearrange("(kc p) f -> p kc f", p=128), [], [t_w], q="pool")
                DMA(wbA[:], w_brA.rearrange("(kc p) f -> p kc f", p=128), [], [t_w], q="pool")
                DMA(wbF[:], w_brF.rearrange("(kc p) f -> p kc f", p=128), [], [t_w], q="pool")
                DMA(wo[:], w_out.rearrange("(kc p) f -> p kc f", p=128), [], [t_w], q="pool")
                for tt in range(4):
                    emit_norm(1, tt, nb, lambda kc: h2[:, kc, :], lambda kc: t_h2[kc])
                    for c in range(8):
                        DMA(OT[:, c, :], OTd.ap()[2 * c:2 * c + 2, :, tok(tt)].rearrange("h d t -> (h d) t"), [tOTd], [t_OT[c]])
                    for fc in range(8):
                        fs = slice(fc * 128, (fc + 1) * 128)
                        for kc in range(8):
                            MM(psA[:], wga[:, kc, fs], h2[:, kc, :], kc == 0, kc == 7, [t_w, t_h2[kc]], [t_psA])
                        for kc in range(8):
                            MM(psB[:], wgb_[:, kc, fs], h2[:, kc, :], kc == 0, kc == 7, [t_w, t_h2[kc]], [t_psB])
                        for c in range(4):
                            MM(psyA[:], wbA[:, c, fs], OT[:, c, :], c == 0, c == 3, [t_w, t_OT[c]], [t_psyA])
                        for c in range(4):
                            MM(psyB[:], wbF[:, c, fs], OT[:, 4 + c, :], c == 0, c == 3, [t_w, t_OT[4 + c]], [t_psyB])
                        ACTV(sA[:], psA[:], AF.Sigmoid, [t_psA], [t_sA])
                        ACTV(sB[:], psB[:], AF.Sigmoid, [t_psB], [t_sB])
                        TT(t1[:], sA[:], psyA[:], ALU.mult, [t_sA, t_psyA], [t_t1])
                        TT(t2[:], sB[:], psyB[:], ALU.mult, [t_sB, t_psyB], [t_t2])
                        TT(mg[:, fc, :], t1[:], t2[:], ALU.add, [t_t1, t_t2], [t_mg[fc]])
                    for f2 in range(8):
                        fs = slice(f2 * 128, (f2 + 1) * 128)
                        for fc in range(8):
                            MM(pso[:], wo[:, fc, fs], mg[:, fc, :], fc == 0, fc == 7, [t_w, t_mg[fc]], [t_pso])
                        STT(xT[:, f2, tok(tt)], pso[:], AG[:, 3, f2:f2 + 1], xT[:, f2, tok(tt)], ALU.mult, ALU.add,
                            [t_pso, tAG], [xT_T[f2][tt]])
                P.barrier()
                P.flush(block)

        if STAGES >= 3:
            phase3()
            if not (DBG & 1):
                phase35()
        if STAGES >= 4:
            xstack.close()
            phase4()
            xstack = ExitStack()
            xT = xstack.enter_context(nc.sbuf_tensor("xT2", [128, 8, NT], F32))
            phase5()

        if STAGES >= 9:
            ffn("f2", 2)

        with ExitStack() as ph:
            Ep = ph.enter_context
            ob = [Ep(nc.sbuf_tensor("ob%d" % i, [128, D], F32)) for i in range(2)]
            tps = [Ep(nc.psum_tensor("otps%d" % i, [128, 512], F32)) for i in range(4)]
            block = Ep(nc.Block())
            t_ob, t_tps = Ts(2), Ts(4)
            t_out = T()
            ev = 0
            for m in range(16):
                b = m % 2
                for half in range(2):
                    pb = (2 * m + half) % 4
                    for q in range(4):
                        fc = half * 4 + q
                        P.op("pe", lambda e, pb=pb, q=q, fc=fc, m=m: e.transpose(
                            tps[pb][:, q * 128:(q + 1) * 128], xT[:, fc, m * 128:(m + 1) * 128], identf[:]),
                            reads=[xT_T[fc][m // 4], tConst], writes=[t_tps[pb]])
                    if ev % 2 == 0:
                        P.op("dve", lambda e, b=b, pb=pb, half=half: e.tensor_copy(
                            out=ob[b][:, half * 512:(half + 1) * 512], in_=tps[pb][:]), reads=[t_tps[pb]], writes=[t_ob[b]])
                    else:
                        P.op("act", lambda e, b=b, pb=pb, half=half: e.activation(
                            out=ob[b][:, half * 512:(half + 1) * 512], in_=tps[pb][:], func=AF.Copy),
                            reads=[t_tps[pb]], writes=[t_ob[b]])
                    ev += 1
                P.dma("sp", lambda e, b=b, m=m: e.dma_start(out=out_d[m * 128:(m + 1) * 128, :], in_=ob[b][:]),
                      reads=[t_ob[b]], writes=[t_out])
            P.barrier()
            P.flush(block)
        xstack.close()
    return nc


def _bf(a):
    return np.ascontiguousarray(a.astype(ml_dtypes.bfloat16))


def make_in_maps(inp):
    f = lambda a: np.ascontiguousarray(np.asarray(a, dtype=np.float32))
    x = f(inp["x"])
    c = f(inp["c"])
    colT = lambda v, n: np.ascontiguousarray(v.reshape(n, 128).T)
    shared = {
        "ada_w": f(inp["ada_w"])[0],
        "ada_bT": colT(f(inp["ada_b"])[0], 72),
        "nT": np.ascontiguousarray(np.stack([colT(f(inp[k])[0], 8) for k in ("norm1_g", "norm2_g", "norm3_g")], axis=1)),
        "f1wg": f(inp["ffn1_wg"])[0], "f1wu": f(inp["ffn1_wu"])[0], "f1wd": f(inp["ffn1_wd"])[0],
        "f2wg": f(inp["ffn2_wg"])[0], "f2wu": f(inp["ffn2_wu"])[0], "f2wd": f(inp["ffn2_wd"])[0],
        "identf": np.eye(128, dtype=np.float32),
        "onesb": _bf(np.ones((128, 128), np.float32)),
        "w_in": f(inp["w_in"])[0],
        "bfT": np.ascontiguousarray(f(inp["b_forget"])[0].reshape(8, 1)),
        "gnT": np.ascontiguousarray(np.tile(np.stack([f(inp[k])[0] for k in ("qn_dsa", "kn_dsa", "qn_fox", "kn_fox")], axis=1), (2, 1))),
        "onesbd": _bf(np.kron(np.eye(2, dtype=np.float32), np.ones((64, 64), np.float32))),
        "w_brA": f(inp["w_br_dsa"])[0], "w_brF": f(inp["w_br_fox"])[0], "w_out": f(inp["w_out"])[0],
        "identb": _bf(np.eye(128, dtype=np.float32)),
    }
    sel = np.zeros((128, 8, 128), np.float32)
    for i in range(8):
        for t16 in range(16):
            for g in range(8):
                sel[i * 16 + t16, g, 16 * g + t16] = 1.0
    shared["sel"] = _bf(sel)
    shared["pow2"] = np.ascontiguousarray(np.tile((0.5 ** np.arange(1, NIT + 1, dtype=np.float64)).astype(np.float32)[None, :], (128, 1)))
    shared["posrow"] = np.ascontiguousarray(np.tile(np.arange(512, dtype=np.float32)[None, :], (128, 1)))
    maps = []
    tq = np.arange(128)[:, None]
    sk = np.arange(128)[None, :]
    for core in range(8):
        b, j = core // 4, core % 4
        xo = x[b].reshape(16, 4, 128, D)[:, j].reshape(NT, D)
        m = dict(shared)
        m["x_own"] = np.ascontiguousarray(xo)
        m["cT"] = colT(c[b], 8)
        vis = np.zeros((128, 4, 128), bool)
        for jp in range(4):
            vis[:, jp, :] = True if jp < j else ((sk <= tq) if jp == j else False)
        m["cmaskf"] = np.ascontiguousarray(np.where(vis, 0.0, -1e30).astype(np.float32).reshape(128, 512))
        m["cmT"] = _bf(np.ascontiguousarray(np.where(vis, 0.0, NEG).astype(np.float32).transpose(2, 1, 0)))
        pos = ((4 * np.arange(16)[:, None] + j) * 128 + np.arange(128)[None, :]).reshape(-1)
        m["posK"] = _bf(np.stack([(pos // 64) * 64, pos % 64]).astype(np.float32))
        sj = np.zeros((8, 4), np.float32)
        sj[:, j] = 1.0
        m["seljF"] = sj
        maps.append(m)
    return maps


_NC = None


def kernel(**inputs):
    global _NC
    if _NC is None:
        _NC = build()
    maps = make_in_maps(inputs)
    res = run_bass_kernel_spmd(_NC, maps, core_ids=list(range(8)))
    out = np.zeros((2, S, D), np.float32)
    for core in range(8):
        b, j = core // 4, core % 4
        o = np.asarray(res.results[core]["out"], dtype=np.float32).reshape(16, 128, D)
        out[b].reshape(16, 4, 128, D)[:, j] = o
    return out
```
